# Optimizing a Trainium2 kernel written in Bass

```python
import jax, jax.numpy as jnp
from jax import lax
import numpy as np

D_MODEL = 2048
BATCH = 4
SEQ = 4096
DEPTH = 1

CHUNK = 64
LEFT_CHUNKS = 8
BAND = LEFT_CHUNKS + 1
ATT_HEADS = 8
ATT_HEAD_DIM = 128
ATT_WIDTH = ATT_HEADS * ATT_HEAD_DIM
REL_CLIP = 128
CONV_WIDTH = 1024
CONV_K = 3
MEM_LEN = 256
MEM_HEADS = 4
MEM_HEAD_DIM = D_MODEL // MEM_HEADS
PEER_HEADS = 8
PEER_NKEYS = 128
PEER_EXPERTS = PEER_NKEYS * PEER_NKEYS
PEER_QDIM = 256
PEER_HALF = PEER_QDIM // 2
PEER_TOPK = 16
PEER_TOKEN_BLOCK = 128
N_BRANCHES = 2
EPS = 1e-6
NEG_INF = -1e30
IN_SIZES = (ATT_WIDTH, ATT_WIDTH, ATT_WIDTH, CONV_WIDTH, CONV_WIDTH, CONV_WIDTH, D_MODEL, D_MODEL)
IN_COLS = sum(IN_SIZES)

kernel_name = 'hybrid_chunked_attn_shortconv_peer_block'


def rmsnorm(x, g):
    xf = x.astype(jnp.float32)
    y = xf * lax.rsqrt(jnp.mean(xf * xf, axis=-1, keepdims=True) + EPS)
    return (y * g.astype(jnp.float32)).astype(x.dtype)


def band_rel_bias(rel_table):
    a = jnp.arange(CHUNK)[:, None, None]
    j = jnp.arange(BAND)[None, :, None]
    b = jnp.arange(CHUNK)[None, None, :]
    rel = (LEFT_CHUNKS - j) * CHUNK + a - b
    idx = jnp.clip(rel, -REL_CLIP, REL_CLIP) + REL_CLIP
    return rel_table[:, idx]


def chunked_band_attention(q, k, v, rel_table):
    bsz, seq = q.shape[0], q.shape[1]
    nc = seq // CHUNK
    shp = (bsz, nc, CHUNK, ATT_HEADS, ATT_HEAD_DIM)
    qc = q.reshape(shp)
    pad = ((0, 0), (LEFT_CHUNKS, 0), (0, 0), (0, 0), (0, 0))
    kp = jnp.pad(k.reshape(shp), pad)
    vp = jnp.pad(v.reshape(shp), pad)
    s = jnp.stack([jnp.einsum('bnqhd,bnkhd->bnhqk', qc, kp[:, j:j + nc]) for j in range(BAND)], axis=4)
    s = s.astype(jnp.float32) * (ATT_HEAD_DIM ** -0.5)
    s = s + band_rel_bias(rel_table).astype(jnp.float32)
    src = jnp.arange(nc)[:, None] + jnp.arange(BAND)[None, :] - LEFT_CHUNKS
    valid = (src >= 0)[None, :, None, None, :, None]
    s = jnp.where(valid, s, NEG_INF)
    p = jax.nn.softmax(s.reshape(bsz, nc, ATT_HEADS, CHUNK, BAND * CHUNK), axis=-1)
    p = p.reshape(bsz, nc, ATT_HEADS, CHUNK, BAND, CHUNK).astype(v.dtype)
    o = jnp.einsum('bnhqk,bnkhd->bnqhd', p[:, :, :, :, 0], vp[:, 0:nc])
    for j in range(1, BAND):
        o = o + jnp.einsum('bnhqk,bnkhd->bnqhd', p[:, :, :, :, j], vp[:, j:j + nc])
    return o.reshape(bsz, seq, ATT_WIDTH)


def causal_depthwise_conv(u, w, b):
    seq = u.shape[1]
    up = jnp.pad(u, ((0, 0), (CONV_K - 1, 0), (0, 0)))
    y = up[:, 0:seq] * w[0] + b
    for i in range(1, CONV_K):
        y = y + up[:, i:i + seq] * w[i]
    return y


def memory_cross_attention(h, mem_n, w_cq, w_ck, w_cv, w_co):
    bsz, seq = h.shape[0], h.shape[1]
    mlen = mem_n.shape[1]
    q = (h @ w_cq).reshape(bsz, seq, MEM_HEADS, MEM_HEAD_DIM)
    k = (mem_n @ w_ck).reshape(bsz, mlen, MEM_HEADS, MEM_HEAD_DIM)
    v = (mem_n @ w_cv).reshape(bsz, mlen, MEM_HEADS, MEM_HEAD_DIM)
    s = jnp.einsum('bshd,bmhd->bhsm', q, k).astype(jnp.float32) * (MEM_HEAD_DIM ** -0.5)
    p = jax.nn.softmax(s, axis=-1).astype(v.dtype)
    o = jnp.einsum('bhsm,bmhd->bshd', p, v).reshape(bsz, seq, D_MODEL)
    return o @ w_co


def peer_ffn(h, w_pq, sub_keys, expert_u, expert_v):
    bsz, seq, dm = h.shape
    q = (h @ w_pq).reshape(bsz, seq, PEER_HEADS, 2, PEER_HALF)
    s = jnp.einsum('bshpd,hpkd->bshpk', q, sub_keys).astype(jnp.float32)
    s1, i1 = lax.top_k(s[:, :, :, 0], PEER_TOPK)
    s2, i2 = lax.top_k(s[:, :, :, 1], PEER_TOPK)
    cand = (s1[..., :, None] + s2[..., None, :]).reshape(bsz, seq, PEER_HEADS, PEER_TOPK * PEER_TOPK)
    cand_idx = (i1[..., :, None] * PEER_NKEYS + i2[..., None, :]).reshape(bsz, seq, PEER_HEADS, PEER_TOPK * PEER_TOPK)
    top_s, pos = lax.top_k(cand, PEER_TOPK)
    idx = jnp.take_along_axis(cand_idx, pos, axis=-1)
    gates = jax.nn.softmax(top_s, axis=-1).astype(h.dtype)
    n_tok = bsz * seq
    n_sel = PEER_HEADS * PEER_TOPK
    nb = n_tok // PEER_TOKEN_BLOCK
    hb = h.reshape(nb, PEER_TOKEN_BLOCK, dm)
    ib = idx.reshape(nb, PEER_TOKEN_BLOCK, n_sel)
    gb = gates.reshape(nb, PEER_TOKEN_BLOCK, n_sel)

    def block(args):
        hx, ix, gx = args
        u_sel = expert_u[ix]
        act = jax.nn.gelu(jnp.einsum('td,ted->te', hx, u_sel), approximate=False) * gx
        v_sel = expert_v[ix]
        return jnp.einsum('te,ted->td', act, v_sel)

    out = lax.map(block, (hb, ib, gb))
    return out.reshape(bsz, seq, dm)


def setup_inputs(seed: int = 0) -> dict:
    key = jax.random.key(seed)
    ks = jax.random.split(key, 24)

    def nrm(k, shape, scale):
        return jax.random.normal(k, shape, jnp.float32) * scale

    def gain(k, shape):
        return 1.0 + 0.05 * jax.random.normal(k, shape, jnp.float32)

    L, D = DEPTH, D_MODEL
    return {
        'x': nrm(ks[0], (BATCH, SEQ, D), 1.0),
        'mem': nrm(ks[1], (BATCH, MEM_LEN, D), 1.0),
        'norm_mix': gain(ks[2], (L, D)),
        'w_in': nrm(ks[3], (L, D, IN_COLS), D ** -0.5),
        'conv_w': nrm(ks[4], (L, CONV_K, CONV_WIDTH), CONV_K ** -0.5),
        'conv_b': nrm(ks[5], (L, CONV_WIDTH), 0.02),
        'rel_bias': nrm(ks[6], (L, ATT_HEADS, 2 * REL_CLIP + 1), 0.5),
        'w_att_out': nrm(ks[7], (L, ATT_WIDTH, D), ATT_WIDTH ** -0.5),
        'w_conv_out': nrm(ks[8], (L, CONV_WIDTH, D), CONV_WIDTH ** -0.5),
        'w_mix_out': nrm(ks[9], (L, D, D), D ** -0.5),
        'norm_cross': gain(ks[10], (L, D)),
        'norm_mem': gain(ks[11], (L, D)),
        'w_cq': nrm(ks[12], (L, D, D), D ** -0.5),
        'w_ck': nrm(ks[13], (L, D, D), D ** -0.5),
        'w_cv': nrm(ks[14], (L, D, D), D ** -0.5),
        'w_co': nrm(ks[15], (L, D, D), D ** -0.5),
        'norm_peer': gain(ks[16], (L, D)),
        'w_pq': nrm(ks[17], (L, D, PEER_HEADS * PEER_QDIM), D ** -0.5),
        'sub_keys': nrm(ks[18], (L, PEER_HEADS, 2, PEER_NKEYS, PEER_HALF), PEER_HALF ** -0.5),
        'expert_u': nrm(ks[19], (L, PEER_EXPERTS, D), D ** -0.5),
        'expert_v': nrm(ks[20], (L, PEER_EXPERTS, D), PEER_HEADS ** -0.5),
        'norm_final': gain(ks[21], (D,)),
    }


def reference(x, mem, norm_mix, w_in, conv_w, conv_b, rel_bias, w_att_out, w_conv_out, w_mix_out,
              norm_cross, norm_mem, w_cq, w_ck, w_cv, w_co, norm_peer, w_pq, sub_keys,
              expert_u, expert_v, norm_final):
    bsz, seq = x.shape[0], x.shape[1]
    split_at = [int(c) for c in np.cumsum(IN_SIZES)[:-1]]
    for l in range(DEPTH):
        h = rmsnorm(x, norm_mix[l])
        proj = h @ w_in[l]
        q, k, v, u, bgate, cgate, ga, gb = jnp.split(proj, split_at, axis=-1)
        hs = (bsz, seq, ATT_HEADS, ATT_HEAD_DIM)
        y_att = chunked_band_attention(q.reshape(hs), k.reshape(hs), v.reshape(hs), rel_bias[l]) @ w_att_out[l]
        y_conv = (bgate * causal_depthwise_conv(cgate * u, conv_w[l], conv_b[l])) @ w_conv_out[l]
        merged = jax.nn.sigmoid(ga) * y_att + jax.nn.sigmoid(gb) * y_conv
        x = x + merged @ w_mix_out[l]
        h = rmsnorm(x, norm_cross[l])
        mem_n = rmsnorm(mem, norm_mem[l])
        x = x + memory_cross_attention(h, mem_n, w_cq[l], w_ck[l], w_cv[l], w_co[l])
        h = rmsnorm(x, norm_peer[l])
        x = x + peer_ffn(h, w_pq[l], sub_keys[l], expert_u[l], expert_v[l])
    return rmsnorm(x, norm_final)
```

```python
import os
import numpy as np
from contextlib import ExitStack
import concourse.bass as bass
import concourse.mybir as mybir
from concourse.bass_utils import run_bass_kernel_spmd

F32 = mybir.dt.float32
BF16 = mybir.dt.bfloat16
U32 = mybir.dt.uint32
AF = mybir.ActivationFunctionType
ALU = mybir.AluOpType
AX = mybir.AxisListType

D = 2048
KT = 16
NT = 2048
HALO = 512
NTH = NT + HALO
EPS = 1e-6
ENG = ("sync", "act", "pool", "dve", "pe")
BLK = {"sync": "sync", "act": "scalar", "pool": "gpsimd", "dve": "vector", "pe": "tensor"}
NS = 16


class Buf:
    __slots__ = ("w", "r", "rd")

    def __init__(self):
        self.w = None
        self.r = {}
        self.rd = []


def bufs(n):
    return [Buf() for _ in range(n)]


class Op:
    __slots__ = ("eng", "fn", "dma", "idx", "deps", "needed", "sem", "val", "prev")


class Sched:
    def __init__(self):
        self.ops = []
        self.last = {e: None for e in ENG}
        self.dmas = []
        self.bdeps = {e: [] for e in ENG}

    def add(self, eng, fn, reads=(), writes=(), dma=False):
        op = Op()
        op.eng, op.fn, op.dma, op.idx, op.needed = eng, fn, dma, len(self.ops), False
        op.sem = None
        op.val = 0
        op.prev = 0
        deps = []
        for b in reads:
            if b.w is not None:
                deps.append(b.w)
        for b in writes:
            if b.w is not None:
                deps.append(b.w)
            deps.extend(b.r.values())
            deps.extend(b.rd)
        if self.bdeps[eng]:
            deps.extend(self.bdeps[eng])
            self.bdeps[eng] = []
        best = {}
        dd = {}
        for d in deps:
            if d.dma:
                dd[d.idx] = d
            else:
                if d.eng == "pe" and eng == "pe" and not dma:
                    continue
                if d.eng not in best or best[d.eng].idx < d.idx:
                    best[d.eng] = d
        op.deps = list(best.values()) + list(dd.values())
        for d in op.deps:
            d.needed = True
        for b in reads:
            if dma:
                b.rd.append(op)
            else:
                b.r[eng] = op
        for b in writes:
            b.w = op
            b.r = {}
            b.rd = []
        self.ops.append(op)
        self.last[eng] = op
        if dma:
            self.dmas.append(op)
        return op

    def barrier(self):
        deps = [o for o in self.last.values() if o is not None] + self.dmas
        for e in ENG:
            self.bdeps[e] = list(deps)
        self.dmas = []

    def emit(self, nc, es):
        EPOCH = 2000
        esem = {e: [] for e in ENG}
        dsem = {e: [es.enter_context(nc.semaphore("d_%s_%d" % (e, i))) for i in range(NS)]
                for e in ("sync", "pool", "act")}
        cnt = {e: 0 for e in ENG}
        dcnt = {e: 0 for e in dsem}
        for op in self.ops:
            if op.dma:
                k = dcnt[op.eng]
                dcnt[op.eng] += 1
                op.sem = dsem[op.eng][k % NS]
                op.val = 16 * (k // NS + 1)
                op.prev = 16 * (k // NS)
            elif op.needed:
                c = cnt[op.eng]
                cnt[op.eng] += 1
                ep = c // EPOCH
                while len(esem[op.eng]) <= ep:
                    esem[op.eng].append(es.enter_context(nc.semaphore("s_%s_%d" % (op.eng, len(esem[op.eng])))))
                op.sem = esem[op.eng][ep]
                op.val = c % EPOCH + 1
        block = es.enter_context(nc.Block())
        for e in ENG:
            stream = [op for op in self.ops if op.eng == e]

            def body(eo, stream=stream, e=e):
                waited = {}

                def wait(sem, val):
                    k = id(sem)
                    if waited.get(k, 0) >= val:
                        return
                    eo.wait_ge(sem, val)
                    waited[k] = val

                for op in stream:
                    for d in op.deps:
                        wait(d.sem, d.val)
                    if op.dma and op.prev > 0:
                        wait(op.sem, op.prev)
                    ins = op.fn(eo)
                    if op.dma:
                        ins.then_inc(op.sem, 16)
                    elif op.needed:
                        ins.then_inc(op.sem, 1)
                if e in dsem:
                    n = dcnt[e]
                    for i in range(NS):
                        uses = (n - i + NS - 1) // NS if n > i else 0
                        if uses > 0:
                            wait(dsem[e][i], 16 * uses)

            getattr(block, BLK[e])(body)


def build(stop=99, debug=False):
    nc = bass.Bass("TRN2", target_bir_lowering=False)
    S = Sched()
    es = ExitStack()

    def din(name, shape, dt=F32):
        return nc.dram_tensor(name, list(shape), dt, kind="ExternalInput").ap()

    def dscr(name, shape, dt, out=False):
        if out:
            return nc.dram_tensor(name, list(shape), dt, kind="ExternalOutput").ap()
        return nc.dram_tensor(name, list(shape), dt).ap()

    def sb(name, shape, dt, stack=None):
        return (stack or es).enter_context(nc.sbuf_tensor("sb_" + name, list(shape), dt))

    xh = din("xh", [NTH, D])
    mem = din("mem", [256, D])
    w_in = din("w_in", [D, 10240])
    w_att_out = din("w_att_out", [1024, D])
    w_conv_out = din("w_conv_out", [1024, D])
    w_mix_out = din("w_mix_out", [D, D])
    w_cq = din("w_cq", [D, D])
    w_ck = din("w_ck", [D, D])
    w_cv = din("w_cv", [D, D])
    w_co = din("w_co", [D, D])
    w_pq = din("w_pq", [D, D])
    gcols_d = din("gcols", [128, 64])
    convp_d = din("convp", [128, 32])
    gfin_d = din("gfin", [128, D])
    biasT_d = din("biasT", [8, 8, 128, 512])
    hv_d = din("hv", [128, 1])
    skT_d = din("skT", [128, 16, 128])
    euT = din("expert_uT", [32, 128, KT * 512])
    ev = din("expert_v", [32, 128, 4 * D])
    identf_d = din("identf", [128, 128])
    iota_d = din("iota", [128, 128])

    out_d = dscr("out", [NT, D], F32, out=True)
    qT_d = dscr("qT_d", [8, 128, NT], BF16)
    kT_d = dscr("kT_d", [8, 128, NTH], BF16)
    v_d = dscr("v_d", [NTH, 1024], BF16)
    sga_d = dscr("sga_d", [16, 128, NT], BF16)
    sgb_d = dscr("sgb_d", [16, 128, NT], BF16)
    x1_d = dscr("x1_d", [NT, D], F32, out=debug)
    x2_d = dscr("x2_d", [NT, D], F32, out=debug)
    ocT_d = dscr("ocT_d", [16, 128, NT], BF16)
    WsD = dscr("WsD", [128, 128, NT], BF16)

    def wview(w):
        return w.rearrange("(kt p) c -> p kt c", p=128)

    identf = sb("identf", [128, 128], F32)
    identb = sb("identb", [128, 128], BF16)
    onesb = sb("onesb", [128, 128], BF16)
    iota = sb("iota", [128, 128], F32)
    gcols = sb("gcols", [128, 64], F32)
    convp = sb("convp", [128, 32], F32)
    hv = sb("hv", [128, 1], F32)
    ss = sb("ss", [128, 32], F32)
    ss2 = sb("ss2", [128, 32], F32)
    rs = sb("rs", [128, 32], F32)
    HO = sb("HO", [128, KT, NT], BF16)
    R = sb("R", [128, 32800], BF16)
    ps = [es.enter_context(nc.psum_tensor("ps%d" % i, [128, 512], F32)) for i in range(8)]
    Bps = bufs(8)
    B_const = Buf()
    B_HO = bufs(16)
    B_ss = bufs(32)
    B_ss2 = bufs(32)
    B_rs = bufs(32)

    def dma(eng, out, in_, reads=(), writes=()):
        return S.add(eng, lambda e: e.dma_start(out=out, in_=in_), reads, writes, dma=True)

    dma("sync", identf[:], identf_d[:, :], writes=[B_const])
    dma("pool", identb[:], identf_d[:, :], writes=[B_const])
    dma("sync", iota[:], iota_d[:, :], writes=[B_const])
    dma("sync", gcols[:], gcols_d[:, :], writes=[B_const])
    dma("sync", convp[:], convp_d[:, :], writes=[B_const])
    dma("sync", hv[:], hv_d[:, :], writes=[B_const])
    S.add("dve", lambda e: e.memset(onesb[:], 1.0), writes=[B_const])
    S.barrier()

    def mm_group(out_ap, pairs, reads, writes):
        def fn(e):
            n = len(pairs)
            ins = None
            for j, (l, r) in enumerate(pairs):
                ins = e.matmul(out_ap, l, r, start=(j == 0), stop=(j == n - 1))
            return ins
        return S.add("pe", fn, reads, writes)

    def norm_phase(stk, ntiles, src_fn, src_bufs_fn, dst_fn, gidx, tag):
        xt = [sb("xt%s%d" % (tag, i), [128, D], F32, stk) for i in range(2)]
        xs = [sb("xs%s%d" % (tag, i), [128, D], BF16, stk) for i in range(2)]
        junk = sb("junk" + tag, [128, D], BF16, stk)
        Bxt, Bxs, Bj = bufs(2), bufs(2), Buf()
        for i in range(ntiles):
            s_ = i % 2
            c = i % 32
            dma("sync", xt[s_][:], src_fn(i), reads=src_bufs_fn(i), writes=[Bxt[s_]])
            S.add("act", lambda e, s_=s_, c=c: e.activation(out=junk[:], in_=xt[s_][:], func=AF.Square,
                                                             accum_out=ss[:, c:c + 1]),
                  reads=[Bxt[s_]], writes=[Bj, B_ss[c]])
            S.add("act", lambda e, c=c: e.activation(out=ss2[:, c:c + 1], in_=ss[:, c:c + 1], func=AF.Sqrt,
                                                     scale=1.0 / D, bias=EPS),
                  reads=[B_ss[c]], writes=[B_ss2[c]])
            S.add("dve", lambda e, c=c: e.reciprocal(out=rs[:, c:c + 1], in_=ss2[:, c:c + 1]),
                  reads=[B_ss2[c]], writes=[B_rs[c]])
            S.add("act", lambda e, s_=s_, c=c: e.activation(out=xs[s_][:], in_=xt[s_][:], func=AF.Copy,
                                                            scale=rs[:, c:c + 1]),
                  reads=[Bxt[s_], B_rs[c]], writes=[Bxs[s_]])
            dst, dbuf = dst_fn(i)
            for half in range(2):
                p = 4 + s_ * 2 + half
                pv = ps[p][:].bitcast(BF16).rearrange("p (a b) -> p a b", a=8)

                def tfn(e, s_=s_, half=half, pv=pv):
                    ins = None
                    for k in range(8):
                        kt = half * 8 + k
                        ins = e.transpose(out=pv[:, k, :], in_=xs[s_][:, kt * 128:(kt + 1) * 128], identity=identb[:])
                    return ins
                S.add("pe", tfn, reads=[Bxs[s_], B_const], writes=[Bps[p]])
                g0 = gidx * 16 + half * 8
                S.add("dve", lambda e, dst=dst, half=half, pv=pv, g0=g0: e.tensor_tensor(
                    out=dst[:, half * 8:(half + 1) * 8, :], in0=pv,
                    in1=gcols[:, g0:g0 + 8].unsqueeze(2).to_broadcast([128, 8, 128]), op=ALU.mult),
                    reads=[Bps[p], B_const], writes=[dbuf])

    B_HH = bufs(4)
    B_qd = [bufs(4) for _ in range(8)]
    B_kd = [bufs(5) for _ in range(8)]
    B_vd = [bufs(2) for _ in range(20)]
    B_sga = [bufs(4) for _ in range(16)]
    B_sgb = [bufs(4) for _ in range(16)]
    B_u = bufs(8)
    B_cv = bufs(8)
    uT = R[:, 0:8 * 2050].rearrange("p (j t) -> p j t", j=8)
    convT = R[:, 16400:16400 + 8 * 2048].rearrange("p (j t) -> p j t", j=8)
    y_attT = R[:, 0:8 * 2048].rearrange("p (j t) -> p j t", j=8)
    B_ya = [bufs(4) for _ in range(8)]

    stkAH = ExitStack()
    HH = sb("HH", [128, KT, HALO], BF16, stkAH)

    def hdst(i):
        if i < 4:
            return HH[:, :, i * 128:(i + 1) * 128], B_HH[i]
        return HO[:, :, (i - 4) * 128:(i - 3) * 128], B_HO[i - 4]

    with ExitStack() as stk:
        norm_phase(stk, 20, lambda i: xh[i * 128:(i + 1) * 128, :], lambda i: [], hdst, 0, "a")
        S.barrier()

    def hgroup(g):
        if g == 0:
            return (lambda kt: HH[:, kt, :]), B_HH
        return (lambda kt, g=g: HO[:, kt, (g - 1) * 512:g * 512]), B_HO[(g - 1) * 4:g * 4]

    psrot = [0]

    def nextps(n=4):
        p = psrot[0] % n
        psrot[0] += 1
        return p

    if stop >= 1:
        with ExitStack() as stk:
            wbuf = [sb("wbufA%d" % i, [128, KT, 512], BF16, stk) for i in range(2)]
            Bwb = bufs(2)
            tmpc = sb("tmpc", [128, NT], F32, stk)
            Btmp = Buf()
            stg = [sb("stgA%d" % i, [128, 512], BF16, stk) for i in range(4)]
            Bstg = bufs(4)
            stgrot = [0]
            w_in_v = wview(w_in)
            order = [6, 7, 10, 11, 8, 9, 0, 1, 2, 3, 4, 5] + list(range(12, 20))
            for n, cg in enumerate(order):
                wb = wbuf[n % 2]
                bw = Bwb[n % 2]
                dma("pool", wb[:], w_in_v[:, :, cg * 512:(cg + 1) * 512], writes=[bw])
                if cg in (4, 5):
                    for tt in range(20):
                        lh, lb = (HH, B_HH[tt]) if tt < 4 else (HO, B_HO[tt - 4])
                        t0 = (tt % 4) * 128 if tt < 4 else (tt - 4) * 128
                        p = nextps()
                        mm_group(ps[p][:], [(lh[:, kt, t0:t0 + 128], wb[:, kt, :]) for kt in range(KT)],
                                 reads=[bw, lb], writes=[Bps[p]])
                        si = stgrot[0] % 4
                        stgrot[0] += 1
                        S.add("act", lambda e, si=si, p=p: e.activation(out=stg[si][:], in_=ps[p][:], func=AF.Copy),
                              reads=[Bps[p]], writes=[Bstg[si]])
                        dma("sync", v_d[tt * 128:(tt + 1) * 128, (cg - 4) * 512:(cg - 3) * 512], stg[si][:],
                            reads=[Bstg[si]], writes=[B_vd[tt][cg - 4]])
                    continue
                for ct in range(4):
                    if cg in (6, 7, 10, 11, 8, 9):
                        j = (cg % 2) * 4 + ct
                        glist = [1, 2, 3, 4] + ([5] if cg in (6, 7, 10, 11) else [])
                    elif cg in (2, 3):
                        glist = [0, 1, 2, 3, 4]
                    else:
                        glist = [1, 2, 3, 4]
                    for g in glist:
                        p = nextps()
                        if g == 5:
                            N = 2
                            rfn, rb = (lambda kt: HH[:, kt, 510:512]), [B_HH[3]]
                        else:
                            N = 512
                            rfn, rb = hgroup(g)
                        mm_group(ps[p][:, 0:N], [(wb[:, kt, ct * 128:(ct + 1) * 128], rfn(kt)) for kt in range(KT)],
                                 reads=[bw] + list(rb), writes=[Bps[p]])
                        if cg in (6, 7):
                            dst = uT[:, j, 0:2] if g == 5 else uT[:, j, 2 + (g - 1) * 512:2 + g * 512]
                            S.add("act", lambda e, dst=dst, p=p, N=N: e.activation(out=dst, in_=ps[p][:, 0:N], func=AF.Copy),
                                  reads=[Bps[p]], writes=[B_u[j]])
                        elif cg in (10, 11):
                            dst = uT[:, j, 0:2] if g == 5 else uT[:, j, 2 + (g - 1) * 512:2 + g * 512]
                            S.add("dve", lambda e, dst=dst, p=p, N=N: e.tensor_tensor(out=dst, in0=ps[p][:, 0:N], in1=dst, op=ALU.mult),
                                  reads=[Bps[p], B_u[j]], writes=[B_u[j]])
                        elif cg in (8, 9):
                            dst = convT[:, j, (g - 1) * 512:g * 512]
                            S.add("dve", lambda e, dst=dst, p=p: e.tensor_tensor(out=dst, in0=ps[p][:], in1=dst, op=ALU.mult),
                                  reads=[Bps[p], B_cv[j]], writes=[B_cv[j]])
                        else:
                            si = stgrot[0] % 4
                            stgrot[0] += 1
                            func = AF.Sigmoid if cg >= 12 else AF.Copy
                            S.add("act", lambda e, si=si, p=p, func=func: e.activation(out=stg[si][:], in_=ps[p][:], func=func),
                                  reads=[Bps[p]], writes=[Bstg[si]])
                            if cg in (0, 1):
                                h = cg * 4 + ct
                                dma("sync", qT_d[h, :, (g - 1) * 512:g * 512], stg[si][:], reads=[Bstg[si]], writes=[B_qd[h][g - 1]])
                            elif cg in (2, 3):
                                h = (cg - 2) * 4 + ct
                                dma("sync", kT_d[h, :, g * 512:(g + 1) * 512], stg[si][:], reads=[Bstg[si]], writes=[B_kd[h][g]])
                            elif cg < 16:
                                c16 = (cg - 12) * 4 + ct
                                dma("sync", sga_d[c16, :, (g - 1) * 512:g * 512], stg[si][:], reads=[Bstg[si]], writes=[B_sga[c16][g - 1]])
                            else:
                                c16 = (cg - 16) * 4 + ct
                                dma("sync", sgb_d[c16, :, (g - 1) * 512:g * 512], stg[si][:], reads=[Bstg[si]], writes=[B_sgb[c16][g - 1]])
                    if cg in (10, 11):
                        w0 = convp[:, j * 4 + 0:j * 4 + 1]
                        w1 = convp[:, j * 4 + 1:j * 4 + 2]
                        w2 = convp[:, j * 4 + 2:j * 4 + 3]
                        bb = convp[:, j * 4 + 3:j * 4 + 4]
                        S.add("dve", lambda e, j=j, w2=w2, bb=bb: e.tensor_scalar(out=tmpc[:], in0=uT[:, j, 2:2050], scalar1=w2, scalar2=bb,
                                                                               op0=ALU.mult, op1=ALU.add),
                              reads=[B_u[j], B_const], writes=[Btmp])
                        S.add("dve", lambda e, j=j, w1=w1: e.scalar_tensor_tensor(out=tmpc[:], in0=uT[:, j, 1:2049], scalar=w1, in1=tmpc[:],
                                                                                   op0=ALU.mult, op1=ALU.add),
                              reads=[B_u[j], Btmp], writes=[Btmp])
                        S.add("dve", lambda e, j=j, w0=w0: e.scalar_tensor_tensor(out=convT[:, j, :], in0=uT[:, j, 0:2048], scalar=w0, in1=tmpc[:],
                                                                                   op0=ALU.mult, op1=ALU.add),
                              reads=[B_u[j], Btmp], writes=[B_cv[j]])
            S.barrier()
    stkAH.close()

    if stop >= 2:
        with ExitStack() as stk:
            qh = [sb("qh%d" % i, [128, NT], BF16, stk) for i in range(2)]
            kh = [sb("kh%d" % i, [128, NTH], BF16, stk) for i in range(2)]
            vh = [sb("vh%d" % i, [128, 20, 128], BF16, stk) for i in range(2)]
            Mh = [sb("Mh%d" % i, [128, 8, 512], BF16, stk) for i in range(2)]
            bst = sb("bst", [128, 8, 512], F32, stk)
            E = [sb("E%d" % i, [128, 512], F32, stk) for i in range(2)]
            PT = [sb("PT%d" % i, [128, 512], BF16, stk) for i in range(3)]
            rz = [sb("rz%d" % i, [128, 512], F32, stk) for i in range(2)]
            Bq, Bk, Bv, BM, Bbst, BE, BPT, Brz = bufs(2), bufs(2), bufs(2), bufs(2), Buf(), bufs(2), bufs(3), bufs(2)
            scale = 128 ** -0.5
            it = 0
            for h in range(8):
                s_ = h % 2
                dma("sync", qh[s_][:], qT_d[h], reads=B_qd[h], writes=[Bq[s_]])
                dma("sync", kh[s_][:], kT_d[h], reads=B_kd[h], writes=[Bk[s_]])
                dma("sync", vh[s_][:], v_d.rearrange("(tt p) c -> p tt c", p=128)[:, :, h * 128:(h + 1) * 128],
                    reads=[B_vd[tt][h // 4] for tt in range(20)], writes=[Bv[s_]])
                dma("sync", bst[:], biasT_d[h].rearrange("i p q -> p i q"), writes=[Bbst])
                S.add("act", lambda e, s_=s_: e.activation(out=Mh[s_][:], in_=bst[:], func=AF.Exp), reads=[Bbst], writes=[BM[s_]])
                for g in range(4):
                    pO = 4 + (g % 2)
                    pZ = 6 + (g % 2)

                    def s_mm(i, itn):
                        pS = itn % 4
                        kc = g * 512 + i * 128
                        S.add("pe", lambda e, pS=pS, s_=s_, kc=kc, g=g: e.matmul(ps[pS][:], kh[s_][:, kc:kc + 128], qh[s_][:, g * 512:(g + 1) * 512],
                                                                                   start=True, stop=True),
                              reads=[Bk[s_], Bq[s_]], writes=[Bps[pS]])
                    s_mm(0, it)
                    s_mm(1, it + 1)
                    for i in range(8):
                        pS = it % 4
                        ei = it % 2
                        S.add("act", lambda e, ei=ei, pS=pS: e.activation(out=E[ei][:], in_=ps[pS][:], func=AF.Exp, scale=scale),
                              reads=[Bps[pS]], writes=[BE[ei]])
                        pi = it % 3
                        if g == 0 and i < 4:
                            S.add("dve", lambda e, pi=pi, ei=ei, s_=s_, i=i: e.scalar_tensor_tensor(
                                out=PT[pi][:], in0=E[ei][:], scalar=hv[:, 0:1], in1=Mh[s_][:, i, :], op0=ALU.mult, op1=ALU.mult),
                                reads=[BE[ei], BM[s_], B_const], writes=[BPT[pi]])
                        else:
                            S.add("dve", lambda e, pi=pi, ei=ei, s_=s_, i=i: e.tensor_tensor(
                                out=PT[pi][:], in0=E[ei][:], in1=Mh[s_][:, i, :], op=ALU.mult),
                                reads=[BE[ei], BM[s_]], writes=[BPT[pi]])
                        if i + 2 < 8:
                            s_mm(i + 2, it + 2)
                        S.add("pe", lambda e, pO=pO, s_=s_, g=g, i=i, pi=pi: e.matmul(ps[pO][:, :], vh[s_][:, g * 4 + i, :], PT[pi][:],
                                                                                       start=(i == 0), stop=(i == 7)),
                              reads=[Bv[s_], BPT[pi]], writes=[Bps[pO]])
                        S.add("pe", lambda e, pZ=pZ, i=i, pi=pi: e.matmul(ps[pZ][:, :], onesb[:], PT[pi][:], start=(i == 0), stop=(i == 7)),
                              reads=[BPT[pi], B_const], writes=[Bps[pZ]])
                        it += 1
                    ri = g % 2
                    S.add("dve", lambda e, ri=ri, pZ=pZ: e.reciprocal(out=rz[ri][:], in_=ps[pZ][:]), reads=[Bps[pZ]], writes=[Brz[ri]])
                    S.add("dve", lambda e, ri=ri, pO=pO, h=h, g=g: e.tensor_tensor(out=y_attT[:, h, g * 512:(g + 1) * 512], in0=ps[pO][:],
                                                                                   in1=rz[ri][:], op=ALU.mult),
                          reads=[Bps[pO], Brz[ri]], writes=[B_ya[h][g]])
            S.barrier()

    B_x1 = [bufs(4) for _ in range(16)]
    if stop >= 3:
        with ExitStack() as stk:
            wa = [sb("wa%d" % i, [128, 8, 512], BF16, stk) for i in range(2)]
            wc = [sb("wc%d" % i, [128, 8, 512], BF16, stk) for i in range(2)]
            sga_t = [sb("sga%d" % i, [128, NT], BF16, stk) for i in range(2)]
            sgb_t = [sb("sgb%d" % i, [128, NT], BF16, stk) for i in range(2)]
            t1 = [sb("t1_%d" % i, [128, 512], F32, stk) for i in range(2)]
            t2 = [sb("t2_%d" % i, [128, 512], F32, stk) for i in range(2)]
            Bwa, Bwc, Bsa, Bsb, Bt1, Bt2 = bufs(2), bufs(2), bufs(2), bufs(2), bufs(2), bufs(2)
            wav = w_att_out.rearrange("(kt p) c -> p kt c", p=128)
            wcv = w_conv_out.rearrange("(kt p) c -> p kt c", p=128)
            it = 0
            for cg in range(4):
                s_ = cg % 2
                dma("pool", wa[s_][:], wav[:, :, cg * 512:(cg + 1) * 512], writes=[Bwa[s_]])
                dma("pool", wc[s_][:], wcv[:, :, cg * 512:(cg + 1) * 512], writes=[Bwc[s_]])
                for ct in range(4):
                    c16 = cg * 4 + ct
                    s2 = c16 % 2
                    dma("sync", sga_t[s2][:], sga_d[c16], reads=B_sga[c16], writes=[Bsa[s2]])
                    dma("sync", sgb_t[s2][:], sgb_d[c16], reads=B_sgb[c16], writes=[Bsb[s2]])
                    for g in range(4):
                        pA = (it % 2) * 2
                        pB = pA + 1
                        ti = it % 2
                        it += 1
                        gs = slice(g * 512, (g + 1) * 512)
                        mm_group(ps[pA][:], [(wa[s_][:, kt, ct * 128:(ct + 1) * 128], y_attT[:, kt, gs]) for kt in range(8)],
                                 reads=[Bwa[s_]] + [B_ya[kt][g] for kt in range(8)], writes=[Bps[pA]])
                        mm_group(ps[pB][:], [(wc[s_][:, kt, ct * 128:(ct + 1) * 128], convT[:, kt, gs]) for kt in range(8)],
                                 reads=[Bwc[s_]] + B_cv, writes=[Bps[pB]])
                        S.add("dve", lambda e, ti=ti, pA=pA, s2=s2, gs=gs: e.tensor_tensor(out=t1[ti][:], in0=ps[pA][:], in1=sga_t[s2][:, gs], op=ALU.mult),
                              reads=[Bps[pA], Bsa[s2]], writes=[Bt1[ti]])
                        S.add("dve", lambda e, ti=ti, pB=pB, s2=s2, gs=gs: e.tensor_tensor(out=t2[ti][:], in0=ps[pB][:], in1=sgb_t[s2][:, gs], op=ALU.mult),
                              reads=[Bps[pB], Bsb[s2]], writes=[Bt2[ti]])
                        S.add("pool", lambda e, ti=ti, c16=c16, gs=gs: e.tensor_tensor(out=HO[:, c16, gs], in0=t1[ti][:], in1=t2[ti][:], op=ALU.add),
                              reads=[Bt1[ti], Bt2[ti]], writes=B_HO[g * 4:(g + 1) * 4])
            S.barrier()
        with ExitStack() as stk:
            RF = R[:, 0:8192].bitcast(F32)
            xp = [RF[:, i * 512:(i + 1) * 512] for i in range(2)]
            x1p = [RF[:, (2 + i) * 512:(3 + i) * 512] for i in range(2)]
            wm = [sb("wm%d" % i, [128, KT, 512], BF16, stk) for i in range(2)]
            Bwm, Bxp, Bx1p = bufs(2), bufs(2), bufs(2)
            wmv = wview(w_mix_out)
            it = 0
            for cg in range(4):
                s_ = cg % 2
                dma("pool", wm[s_][:], wmv[:, :, cg * 512:(cg + 1) * 512], writes=[Bwm[s_]])
                for tt in range(16):
                    xi = it % 2
                    p = it % 4
                    it += 1
                    dma("sync", xp[xi][:], xh[HALO + tt * 128:HALO + (tt + 1) * 128, cg * 512:(cg + 1) * 512], writes=[Bxp[xi]])
                    mm_group(ps[p][:], [(HO[:, kt, tt * 128:(tt + 1) * 128], wm[s_][:, kt, :]) for kt in range(KT)],
                             reads=[Bwm[s_], B_HO[tt]], writes=[Bps[p]])
                    S.add("dve", lambda e, xi=xi, p=p: e.tensor_tensor(out=x1p[xi][:], in0=ps[p][:], in1=xp[xi][:], op=ALU.add),
                          reads=[Bps[p], Bxp[xi]], writes=[Bx1p[xi]])
                    dma("sync", x1_d[tt * 128:(tt + 1) * 128, cg * 512:(cg + 1) * 512], x1p[xi][:], reads=[Bx1p[xi]], writes=[B_x1[tt][cg]])
            S.barrier()

    B_x2 = [bufs(4) for _ in range(16)]
    B_oc = [bufs(4) for _ in range(16)]
    if stop >= 4:
        stkB = ExitStack()
        memT = sb("memT", [128, KT, 256], BF16, stkB)
        kmT = sb("kmT", [128, 16, 256], BF16, stkB)
        vm = sb("vm", [128, 2, D], BF16, stkB)
        BmemT, BkmT, Bvm = bufs(2), Buf(), Buf()
        with ExitStack() as stk:
            norm_phase(stk, 2, lambda i: mem[i * 128:(i + 1) * 128, :], lambda i: [],
                       lambda i: (memT[:, :, i * 128:(i + 1) * 128], BmemT[i]), 2, "m")
            S.barrier()
        with ExitStack() as stk:
            wbuf = [sb("wbufB%d" % i, [128, KT, 512], BF16, stk) for i in range(2)]
            Bwb = bufs(2)
            n = 0
            import os
            for wsrc, kind in (() if "B0w" in os.environ.get("KSKIP", "") else ((w_ck, 0), (w_cv, 1))):
                wv_ = wview(wsrc)
                for cg in range(4):
                    wb, bw = wbuf[n % 2], Bwb[n % 2]
                    n += 1
                    dma("pool", wb[:], wv_[:, :, cg * 512:(cg + 1) * 512], writes=[bw])
                    if kind == 0:
                        for ct in range(4):
                            p = nextps()
                            mm_group(ps[p][:, 0:256], [(wb[:, kt, ct * 128:(ct + 1) * 128], memT[:, kt, :]) for kt in range(KT)],
                                     reads=[bw] + BmemT, writes=[Bps[p]])
                            S.add("act", lambda e, p=p, c16=cg * 4 + ct: e.activation(out=kmT[:, c16, :], in_=ps[p][:, 0:256], func=AF.Copy),
                                  reads=[Bps[p]], writes=[BkmT])
                    else:
                        for mt in range(2):
                            p = nextps()
                            mm_group(ps[p][:], [(memT[:, kt, mt * 128:(mt + 1) * 128], wb[:, kt, :]) for kt in range(KT)],
                                     reads=[bw] + BmemT, writes=[Bps[p]])
                            S.add("act", lambda e, p=p, mt=mt, cg=cg: e.activation(out=vm[:, mt, cg * 512:(cg + 1) * 512], in_=ps[p][:], func=AF.Copy),
                                  reads=[Bps[p]], writes=[Bvm])
            S.barrier()
        with ExitStack() as stk:
            import os
            norm_phase(stk, 0 if "B1" in os.environ.get("KSKIP", "") else 16, lambda i: x1_d[i * 128:(i + 1) * 128, :], lambda i: B_x1[i],
                       lambda i: (HO[:, :, i * 128:(i + 1) * 128], B_HO[i]), 1, "b")
            S.barrier()
        import os
        SKIP = os.environ.get("KSKIP", "")
        with ExitStack() as stk:
            if "B2" in SKIP:
                raise_skip = True
            wbuf = [sb("wbufQ%d" % i, [128, KT, 512], BF16, stk) for i in range(2)]
            Bwb = bufs(2)
            qcT = R[:, 0:4 * NT].rearrange("p (c t) -> p c t", c=4)
            Bqc = [bufs(4) for _ in range(4)]
            Ec = [sb("Ec%d" % i, [128, 512], BF16, stk) for i in range(4)]
            BEc = bufs(4)
            rzc = [sb("rzc%d" % i, [128, 512], F32, stk) for i in range(2)]
            Brzc = bufs(2)
            stgB2 = [sb("stgB%d" % i, [128, 512], BF16, stk) for i in range(4)]
            BstgB2 = bufs(4)
            wqv = wview(w_cq)
            scaleB = 512 ** -0.5
            it = 0
            si_rot = 0
            for hh in range(0 if "B2" in SKIP else 4):
                wb, bw = wbuf[hh % 2], Bwb[hh % 2]
                dma("pool", wb[:], wqv[:, :, hh * 512:(hh + 1) * 512], writes=[bw])
                for ct in range(4):
                    for g in range(4):
                        p = nextps(2)
                        mm_group(ps[p][:], [(wb[:, kt, ct * 128:(ct + 1) * 128], HO[:, kt, g * 512:(g + 1) * 512]) for kt in range(KT)],
                                 reads=[bw] + B_HO[g * 4:(g + 1) * 4], writes=[Bps[p]])
                        S.add("act", lambda e, p=p, ct=ct, g=g: e.activation(out=qcT[:, ct, g * 512:(g + 1) * 512], in_=ps[p][:], func=AF.Copy),
                              reads=[Bps[p]], writes=[Bqc[ct][g]])
                for g in range(4):
                    gs = slice(g * 512, (g + 1) * 512)
                    eis = []
                    for mt in range(2):
                        p = 2 + (it % 2)
                        ei = it % 4
                        it += 1
                        eis.append(ei)
                        mm_group(ps[p][:], [(kmT[:, hh * 4 + ct, mt * 128:(mt + 1) * 128], qcT[:, ct, gs]) for ct in range(4)],
                                 reads=[BkmT] + [Bqc[ct][g] for ct in range(4)], writes=[Bps[p]])
                        S.add("act", lambda e, p=p, ei=ei: e.activation(out=Ec[ei][:], in_=ps[p][:], func=AF.Exp, scale=scaleB),
                              reads=[Bps[p]], writes=[BEc[ei]])
                    pZ = 4 + (g % 2)
                    ri = g % 2
                    mm_group(ps[pZ][:], [(onesb[:], Ec[eis[mt]][:]) for mt in range(2)], reads=[BEc[e_] for e_ in eis] + [B_const], writes=[Bps[pZ]])
                    S.add("dve", lambda e, ri=ri, pZ=pZ: e.reciprocal(out=rzc[ri][:], in_=ps[pZ][:]), reads=[Bps[pZ]], writes=[Brzc[ri]])
                    for ct in range(4):
                        c16 = hh * 4 + ct
                        pO = 6 + (ct % 2)
                        mm_group(ps[pO][:], [(vm[:, mt, c16 * 128:(c16 + 1) * 128], Ec[eis[mt]][:]) for mt in range(2)],
                                 reads=[Bvm] + [BEc[e_] for e_ in eis], writes=[Bps[pO]])
                        si = si_rot % 4
                        si_rot += 1
                        S.add("dve", lambda e, si=si, pO=pO, ri=ri: e.tensor_tensor(out=stgB2[si][:], in0=ps[pO][:], in1=rzc[ri][:], op=ALU.mult),
                              reads=[Bps[pO], Brzc[ri]], writes=[BstgB2[si]])
                        dma("sync", ocT_d[c16, :, gs], stgB2[si][:], reads=[BstgB2[si]], writes=[B_oc[c16][g]])
            S.barrier()
        stkB.close()
        with ExitStack() as stk:
            RF = R[:, 16384:24576].bitcast(F32)
            xpB3 = [RF[:, i * 512:(i + 1) * 512] for i in range(2)]
            x2p = [RF[:, (2 + i) * 512:(3 + i) * 512] for i in range(2)]
            wbuf = [sb("wbufO%d" % i, [128, KT, 512], BF16, stk) for i in range(2)]
            Bwb = bufs(2)
            ocg = [sb("ocg%d" % i, [128, KT, 512], BF16, stk) for i in range(2)]
            Bocg = bufs(2)
            BxpB3, Bx2p = bufs(2), bufs(2)
            wov = wview(w_co)
            it = 0
            n = 0
            for cg in range(0 if "B3" in SKIP else 4):
                wb, bw = wbuf[cg % 2], Bwb[cg % 2]
                dma("pool", wb[:], wov[:, :, cg * 512:(cg + 1) * 512], writes=[bw])
                for g in range(4):
                    oi = n % 2
                    n += 1
                    dma("sync", ocg[oi][:], ocT_d.rearrange("c p t -> p c t")[:, :, g * 512:(g + 1) * 512],
                        reads=[B_oc[c][g] for c in range(16)], writes=[Bocg[oi]])
                    for t4 in range(4):
                        tt = g * 4 + t4
                        xi = it % 2
                        p = it % 4
                        it += 1
                        dma("sync", xpB3[xi][:], x1_d[tt * 128:(tt + 1) * 128, cg * 512:(cg + 1) * 512], reads=[B_x1[tt][cg]], writes=[BxpB3[xi]])
                        mm_group(ps[p][:], [(ocg[oi][:, kt, t4 * 128:(t4 + 1) * 128], wb[:, kt, :]) for kt in range(KT)],
                                 reads=[bw, Bocg[oi]], writes=[Bps[p]])
                        S.add("dve", lambda e, xi=xi, p=p: e.tensor_tensor(out=x2p[xi][:], in0=ps[p][:], in1=xpB3[xi][:], op=ALU.add),
                              reads=[Bps[p], BxpB3[xi]], writes=[Bx2p[xi]])
                        dma("sync", x2_d[tt * 128:(tt + 1) * 128, cg * 512:(cg + 1) * 512], x2p[xi][:], reads=[Bx2p[xi]], writes=[B_x2[tt][cg]])
            S.barrier()

    if stop >= 5:
        with ExitStack() as stk:
            norm_phase(stk, 16, lambda i: x2_d[i * 128:(i + 1) * 128, :], lambda i: B_x2[i],
                       lambda i: (HO[:, :, i * 128:(i + 1) * 128], B_HO[i]), 3, "c")
            S.barrier()
        B_Ws = bufs(16)
        with ExitStack() as stk:
            wpq = sb("wpq", [128, KT, 512], BF16, stk)
            Bwpq = Buf()
            skT = sb("skT", [128, 16, 128], BF16, stk)
            BskT = Buf()
            qpT = sb("qpT", [128, 16, 512], BF16, stk)
            Bqp = bufs(16)
            s_sb = sb("s_sb", [128, 16, 128], F32, stk)
            Bsh = bufs(16)
            mx = sb("mx", [128, 16, 16], F32, stk)
            ix = sb("ix", [128, 16, 16], U32, stk)
            ixf = sb("ixf", [128, 16, 16], F32, stk)
            Bmx, Bix, Bixf = bufs(16), bufs(16), Buf()
            cand = sb("cand", [128, 8, 256], F32, stk)
            Bch = bufs(8)
            top = sb("top", [128, 8, 16], F32, stk)
            pos = sb("pos", [128, 8, 16], U32, stk)
            Btop, Bpos = bufs(8), bufs(8)
            ir = sb("ir", [128, 128], U32, stk)
            jr = sb("jr", [128, 128], U32, stk)
            irf = sb("irf", [128, 8, 16], F32, stk)
            jrf = sb("jrf", [128, 8, 16], F32, stk)
            oh = sb("oh", [128, 8, 16, 16], F32, stk)
            Boh = Buf()
            negm = sb("negm", [128, 8], F32, stk)
            Z = sb("Z", [128, 8], F32, stk)
            rZ = sb("rZ", [128, 8], F32, stk)
            et = sb("et", [128, 8, 16], F32, stk)
            Bmisc = bufs(8)
            tok3 = sb("tok3", [128, 3, 128], F32, stk)
            Btok3 = bufs(3)
            slot3s = [sb("slot3_%d" % i, [128, 3, 128], F32, stk) for i in range(2)]
            Bslot3s = bufs(2)
            P2s = [R[:, i * 4096:(i + 1) * 4096].rearrange("p (t k) -> p t k", t=32) for i in range(2)]
            P1s = [R[:, 8192 + i * 4096:8192 + (i + 1) * 4096].rearrange("p (t k) -> p t k", t=32) for i in range(2)]
            BP2s, BP1s = bufs(2), bufs(2)
            pq_rot = [0]
            wrot = [0]
            WT = R[:, 16384:32768].rearrange("p (k t) -> p k t", k=128)
            BWT = Buf()
            dma("pool", skT[:], skT_d[:, :, :], writes=[BskT])
            wpv = wview(w_pq)
            iota_b = iota[:, 0:16]
            def projection(g):
                gsl = slice(g * 512, (g + 1) * 512)
                for cg in range(4):
                    dma("pool", wpq[:], wpv[:, :, cg * 512:(cg + 1) * 512], writes=[Bwpq])
                    for ct in range(4):
                        p = nextps(2)
                        hp = cg * 4 + ct
                        mm_group(ps[p][:], [(wpq[:, kt, ct * 128:(ct + 1) * 128], HO[:, kt, gsl]) for kt in range(KT)],
                                 reads=[Bwpq] + B_HO[g * 4:(g + 1) * 4], writes=[Bps[p]])
                        S.add("act", lambda e, p=p, hp=hp: e.activation(out=qpT[:, hp, :], in_=ps[p][:], func=AF.Copy),
                              reads=[Bps[p]], writes=[Bqp[hp]])
            def stageA(g, t4, sl):
                tt = g * 4 + t4
                tsl = slice(t4 * 128, (t4 + 1) * 128)
                for b in range(4):
                    p = 2 + (b % 2)

                    def sfn(e, p=p, b=b, tsl=tsl):
                        ins = None
                        for q in range(4):
                            hp = b * 4 + q
                            ins = e.matmul(ps[p][:, q * 128:(q + 1) * 128], qpT[:, hp, tsl], skT[:, hp, :], start=True, stop=True)
                        return ins
                    S.add("pe", sfn, reads=Bqp[b * 4:(b + 1) * 4] + [BskT], writes=[Bps[p]])
                    S.add("act", lambda e, p=p, b=b: e.activation(out=s_sb[:, b * 4:(b + 1) * 4, :].rearrange("p a b -> p (a b)"),
                                                                  in_=ps[p][:], func=AF.Copy),
                          reads=[Bps[p]], writes=Bsh[b * 4:(b + 1) * 4])
                for hp in range(16):
                    S.add("dve", lambda e, hp=hp: e.max(out=mx[:, hp, 0:8], in_=s_sb[:, hp, :]), reads=[Bsh[hp]], writes=[Bmx[hp]])
                for hp in range(16):
                    S.add("dve", lambda e, hp=hp: e.max_index(out=ix[:, hp, 0:8], in_max=mx[:, hp, 0:8], in_values=s_sb[:, hp, :]),
                          reads=[Bsh[hp], Bmx[hp]], writes=[Bix[hp]])
                for hp in range(16):
                    S.add("dve", lambda e, hp=hp: e.match_replace(out=s_sb[:, hp, :], in_to_replace=mx[:, hp, 0:8], in_values=s_sb[:, hp, :],
                                                                  imm_value=-1e30),
                          reads=[Bmx[hp]], writes=[Bsh[hp]])
                for hp in range(16):
                    S.add("dve", lambda e, hp=hp: e.max(out=mx[:, hp, 8:16], in_=s_sb[:, hp, :]), reads=[Bsh[hp]], writes=[Bmx[hp]])
                for hp in range(16):
                    S.add("dve", lambda e, hp=hp: e.max_index(out=ix[:, hp, 8:16], in_max=mx[:, hp, 8:16], in_values=s_sb[:, hp, :]),
                          reads=[Bsh[hp], Bmx[hp]], writes=[Bix[hp]])
                S.add("dve", lambda e: e.tensor_copy(out=ixf[:], in_=ix[:]), reads=Bix, writes=[Bixf])
                mxv = mx[:].rearrange("p (h two) k -> p h two k", two=2)
                for hh2 in range(2):
                    S.add("dve", lambda e, mxv=mxv, hh2=hh2: e.tensor_tensor(
                        out=cand[:, hh2 * 4:(hh2 + 1) * 4, :].rearrange("p h (i j) -> p h i j", i=16),
                        in0=mxv[:, hh2 * 4:(hh2 + 1) * 4, 0, :].unsqueeze(3).to_broadcast([128, 4, 16, 16]),
                        in1=mxv[:, hh2 * 4:(hh2 + 1) * 4, 1, :].unsqueeze(2).to_broadcast([128, 4, 16, 16]), op=ALU.add),
                        reads=Bmx, writes=Bch[hh2 * 4:(hh2 + 1) * 4])
                for h in range(8):
                    S.add("dve", lambda e, h=h: e.max(out=top[:, h, 0:8], in_=cand[:, h, :]), reads=[Bch[h]], writes=[Btop[h]])
                for h in range(8):
                    S.add("dve", lambda e, h=h: e.max_index(out=pos[:, h, 0:8], in_max=top[:, h, 0:8], in_values=cand[:, h, :]),
                          reads=[Bch[h], Btop[h]], writes=[Bpos[h]])
                for h in range(8):
                    S.add("dve", lambda e, h=h: e.match_replace(out=cand[:, h, :], in_to_replace=top[:, h, 0:8], in_values=cand[:, h, :],
                                                                imm_value=-1e30),
                          reads=[Btop[h]], writes=[Bch[h]])
                for h in range(8):
                    S.add("dve", lambda e, h=h: e.max(out=top[:, h, 8:16], in_=cand[:, h, :]), reads=[Bch[h]], writes=[Btop[h]])
                for h in range(8):
                    S.add("dve", lambda e, h=h: e.max_index(out=pos[:, h, 8:16], in_max=top[:, h, 8:16], in_values=cand[:, h, :]),
                          reads=[Bch[h], Btop[h]], writes=[Bpos[h]])
                posf = pos[:].rearrange("p h r -> p (h r)")
                S.add("dve", lambda e, posf=posf: e.tensor_single_scalar(out=ir[:], in_=posf, scalar=4, op=ALU.logical_shift_right),
                      reads=Bpos, writes=[Bmisc[0]])
                S.add("dve", lambda e, posf=posf: e.tensor_single_scalar(out=jr[:], in_=posf, scalar=15, op=ALU.bitwise_and),
                      reads=Bpos, writes=[Bmisc[1]])
                S.add("dve", lambda e: e.tensor_copy(out=irf[:].rearrange("p h r -> p (h r)"), in_=ir[:]), reads=[Bmisc[0]], writes=[Bmisc[2]])
                S.add("dve", lambda e: e.tensor_copy(out=jrf[:].rearrange("p h r -> p (h r)"), in_=jr[:]), reads=[Bmisc[1]], writes=[Bmisc[3]])
                ixv = ixf[:].rearrange("p (h two) k -> p h two k", two=2)
                for which, rf, bm in ((0, irf, Bmisc[2]), (1, jrf, Bmisc[3])):
                    S.add("dve", lambda e, rf=rf: e.tensor_tensor(
                        out=oh[:], in0=rf[:].unsqueeze(3).to_broadcast([128, 8, 16, 16]),
                        in1=iota_b.unsqueeze(1).unsqueeze(1).to_broadcast([128, 8, 16, 16]), op=ALU.is_equal),
                        reads=[bm, B_const], writes=[Boh])
                    S.add("dve", lambda e, which=which, ixv=ixv: e.tensor_tensor(
                        out=oh[:], in0=oh[:], in1=ixv[:, :, which, :].unsqueeze(2).to_broadcast([128, 8, 16, 16]), op=ALU.mult),
                        reads=[Boh, Bixf], writes=[Boh])
                    S.add("dve", lambda e, which=which: e.tensor_reduce(
                        out=tok3[:, which, :], in_=oh[:].rearrange("p h r i -> p (h r) i"), axis=AX.X, op=ALU.add),
                        reads=[Boh], writes=[Btok3[which]])
                S.add("dve", lambda e: e.tensor_scalar(out=negm[:], in0=top[:, :, 0], scalar1=-1.0, scalar2=None, op0=ALU.mult),
                      reads=Btop, writes=[Bmisc[4]])
                for h in range(8):
                    S.add("act", lambda e, h=h: e.activation(out=et[:, h, :], in_=top[:, h, :], func=AF.Exp, bias=negm[:, h:h + 1],
                                                             accum_out=Z[:, h:h + 1]),
                          reads=[Btop[h], Bmisc[4]], writes=[Bmisc[5]])
                S.add("dve", lambda e: e.reciprocal(out=rZ[:], in_=Z[:]), reads=[Bmisc[5]], writes=[Bmisc[6]])
                S.add("dve", lambda e: e.tensor_tensor(out=tok3[:, 2, :].rearrange("p (h r) -> p h r", h=8), in0=et[:],
                                                       in1=rZ[:].unsqueeze(2).to_broadcast([128, 8, 16]), op=ALU.mult),
                      reads=[Bmisc[5], Bmisc[6]], writes=[Btok3[2]])
                p = 4

                def trfn(e, p=p):
                    ins = None
                    for w_ in range(3):
                        ins = e.transpose(out=ps[p][:, w_ * 128:(w_ + 1) * 128], in_=tok3[:, w_, :], identity=identf[:])
                    return ins
                S.add("pe", trfn, reads=Btok3 + [B_const], writes=[Bps[p]])
                S.add("act", lambda e, p=p: e.activation(out=slot3s[sl][:].rearrange("p a b -> p (a b)"), in_=ps[p][:, 0:384], func=AF.Copy),
                      reads=[Bps[p]], writes=[Bslot3s[sl]])
            def stageB(tt, sl):
                for qt in range(4):
                    hs = slice(qt * 32, (qt + 1) * 32)
                    pb = pq_rot[0] % 2
                    pq_rot[0] += 1
                    P2 = P2s[pb]
                    P1 = P1s[pb]
                    S.add("dve", lambda e, hs=hs, P2=P2: e.tensor_tensor(
                        out=P2, in0=iota[:].unsqueeze(1).to_broadcast([128, 32, 128]),
                        in1=slot3s[sl][:, 1, hs].unsqueeze(2).to_broadcast([128, 32, 128]), op=ALU.is_equal),
                        reads=[Bslot3s[sl], B_const], writes=[BP2s[pb]])
                    S.add("dve", lambda e, hs=hs, P1=P1: e.tensor_tensor(
                        out=P1, in0=iota[:].unsqueeze(1).to_broadcast([128, 32, 128]),
                        in1=slot3s[sl][:, 0, hs].unsqueeze(2).to_broadcast([128, 32, 128]), op=ALU.is_equal),
                        reads=[Bslot3s[sl], B_const], writes=[BP1s[pb]])
                    S.add("pool", lambda e, hs=hs, P1=P1: e.tensor_tensor(
                        out=P1, in0=P1, in1=slot3s[sl][:, 2, hs].unsqueeze(2).to_broadcast([128, 32, 128]), op=ALU.mult),
                        reads=[Bslot3s[sl], BP1s[pb]], writes=[BP1s[pb]])
                    for q4 in range(8):
                        p = 5 + (wrot[0] % 3)
                        wrot[0] += 1

                        def wfn(e, p=p, q4=q4, P1=P1, P2=P2):
                            ins = None
                            pv = ps[p][:].rearrange("p (k t) -> p k t", t=4)
                            for q in range(4):
                                tl = q4 * 4 + q
                                ins = e.matmul(pv[:, :, q], P2[:, tl, :], P1[:, tl, :], start=True, stop=True)
                            return ins
                        S.add("pe", wfn, reads=[BP1s[pb], BP2s[pb]], writes=[Bps[p]])
                        t0 = qt * 32 + q4 * 4
                        S.add("act", lambda e, p=p, t0=t0: e.activation(
                            out=WT[:, :, t0:t0 + 4],
                            in_=ps[p][:].rearrange("p (k t) -> p k t", t=4), func=AF.Copy),
                            reads=[Bps[p]], writes=[BWT])
                Wv = WsD.rearrange("k1 k2 t -> k2 k1 t")
                for q8 in range(8):
                    dma("sync", Wv[:, q8 * 16:(q8 + 1) * 16, tt * 128:(tt + 1) * 128], WT[:, q8 * 16:(q8 + 1) * 16, :],
                        reads=[BWT], writes=[B_Ws[tt]])
            tiles = [(g, t4) for g in range(int(os.environ.get("KC1", "4"))) for t4 in range(4)]
            for n_, (g, t4) in enumerate(tiles):
                if t4 == 0:
                    projection(g)
                stageA(g, t4, n_ % 2)
                if n_ >= 1:
                    stageB(tiles[n_ - 1][0] * 4 + tiles[n_ - 1][1], (n_ - 1) % 2)
            if tiles:
                stageB(tiles[-1][0] * 4 + tiles[-1][1], (len(tiles) - 1) % 2)
            S.barrier()
        with ExitStack() as stk:
            ucT = [R[:, i * 8192:(i + 1) * 8192].rearrange("p (kt c) -> p kt c", kt=KT) for i in range(2)]
            vc = [R[:, 16384 + i * 8192:16384 + (i + 1) * 8192].rearrange("p (k d) -> p k d", k=4) for i in range(2)]
            Buc, Bvc = bufs(2), bufs(2)
            oacc = sb("oacc", [128, 4, D], F32, stk)
            Boacc = [bufs(4) for _ in range(4)]
            wch = [sb("wch%d" % i, [128, 4, 512], BF16, stk) for i in range(2)]
            Bwch = bufs(2)
            ge = [sb("ge%d" % i, [128, 512], F32, stk) for i in range(2)]
            Bge = bufs(2)
            actd = [sb("actd%d" % i, [128, 4, 512], BF16, stk) for i in range(2)]
            Bactd = bufs(2)
            xq = [sb("xq%d" % i, [128, 512], F32, stk) for i in range(2)]
            gq = [sb("gq%d" % i, [128, 512], F32, stk) for i in range(2)]
            Bxq, Bgq = bufs(2), bufs(2)
            junk = sb("junkF", [128, D], BF16, stk)
            Bj = Buf()
            arot = [0]
            orot = [0]
            fi = 0
            for g in range(int(os.environ.get("KC4", "4"))):
                gsl = slice(g * 512, (g + 1) * 512)
                def loads(c):
                    s_ = c % 2
                    dma("pool", R[:, s_ * 8192:(s_ + 1) * 8192], euT[c], writes=[Buc[s_]])
                    dma("pool", R[:, 16384 + s_ * 8192:16384 + (s_ + 1) * 8192], ev[c], writes=[Bvc[s_]])
                    dma("sync", wch[s_][:], WsD[c * 4:(c + 1) * 4, :, gsl].rearrange("k p t -> p k t"),
                        reads=B_Ws[g * 4:(g + 1) * 4], writes=[Bwch[s_]])

                def a_group(c, k):
                    s_ = c % 2
                    p = arot[0] % 2
                    gi = arot[0] % 2
                    arot[0] += 1
                    mm_group(ps[p][:], [(ucT[s_][:, kt, k * 128:(k + 1) * 128], HO[:, kt, gsl]) for kt in range(KT)],
                             reads=[Buc[s_]] + B_HO[g * 4:(g + 1) * 4], writes=[Bps[p]])
                    S.add("act", lambda e, p=p, gi=gi: e.activation(out=ge[gi][:], in_=ps[p][:], func=AF.Gelu),
                          reads=[Bps[p]], writes=[Bge[gi]])
                    S.add("dve", lambda e, gi=gi, s_=s_, k=k: e.tensor_tensor(out=actd[s_][:, k, :], in0=ge[gi][:], in1=wch[s_][:, k, :], op=ALU.mult),
                          reads=[Bge[gi], Bwch[s_]], writes=[Bactd[s_]])

                def o_group(c, j):
                    s_ = c % 2
                    t4, dg = j // 4, j % 4
                    p = 2 + (orot[0] % 6)
                    orot[0] += 1
                    dsl = slice(dg * 512, (dg + 1) * 512)
                    mm_group(ps[p][:], [(actd[s_][:, k, t4 * 128:(t4 + 1) * 128], vc[s_][:, k, dsl]) for k in range(4)],
                             reads=[Bactd[s_], Bvc[s_]], writes=[Bps[p]])
                    if c == 0:
                        S.add("dve", lambda e, p=p, t4=t4, dsl=dsl: e.tensor_copy(out=oacc[:, t4, dsl], in_=ps[p][:]),
                              reads=[Bps[p]], writes=[Boacc[t4][dg]])
                    else:
                        S.add("dve", lambda e, p=p, t4=t4, dsl=dsl: e.tensor_tensor(out=oacc[:, t4, dsl], in0=ps[p][:], in1=oacc[:, t4, dsl], op=ALU.add),
                              reads=[Bps[p], Boacc[t4][dg]], writes=[Boacc[t4][dg]])

                loads(0)
                for k in range(4):
                    a_group(0, k)
                for c in range(32):
                    if c + 1 < 32:
                        loads(c + 1)
                    for j in range(16):
                        if j % 4 == 0 and c + 1 < 32:
                            a_group(c + 1, j // 4)
                        o_group(c, j)
                for t4 in range(4):
                    tt = g * 4 + t4
                    c = 16 + (tt % 16)
                    for dg in range(4):
                        dsl = slice(dg * 512, (dg + 1) * 512)
                        xi = fi % 2
                        fi += 1
                        dma("sync", xq[xi][:], x2_d[tt * 128:(tt + 1) * 128, dsl], reads=[B_x2[tt][dg]], writes=[Bxq[xi]])
                        S.add("dve", lambda e, t4=t4, dsl=dsl, xi=xi: e.tensor_tensor(out=oacc[:, t4, dsl], in0=oacc[:, t4, dsl], in1=xq[xi][:], op=ALU.add),
                              reads=[Bxq[xi], Boacc[t4][dg]], writes=[Boacc[t4][dg]])
                    S.add("act", lambda e, t4=t4, c=c: e.activation(out=junk[:], in_=oacc[:, t4, :], func=AF.Square, accum_out=ss[:, c:c + 1]),
                          reads=Boacc[t4], writes=[Bj, B_ss[c]])
                    S.add("act", lambda e, c=c: e.activation(out=ss2[:, c:c + 1], in_=ss[:, c:c + 1], func=AF.Sqrt, scale=1.0 / D, bias=EPS),
                          reads=[B_ss[c]], writes=[B_ss2[c]])
                    S.add("dve", lambda e, c=c: e.reciprocal(out=rs[:, c:c + 1], in_=ss2[:, c:c + 1]), reads=[B_ss2[c]], writes=[B_rs[c]])
                    for dg in range(4):
                        dsl = slice(dg * 512, (dg + 1) * 512)
                        xi = fi % 2
                        fi += 1
                        dma("sync", gq[xi][:], gfin_d[:, dsl], writes=[Bgq[xi]])
                        S.add("dve", lambda e, t4=t4, dsl=dsl, xi=xi, c=c: e.scalar_tensor_tensor(
                            out=oacc[:, t4, dsl], in0=oacc[:, t4, dsl], scalar=rs[:, c:c + 1], in1=gq[xi][:], op0=ALU.mult, op1=ALU.mult),
                            reads=[Bgq[xi], Boacc[t4][dg], B_rs[c]], writes=[Boacc[t4][dg]])
                    dma("sync", out_d[tt * 128:(tt + 1) * 128, :], oacc[:, t4, :], reads=Boacc[t4], writes=[])
            S.barrier()

    S.emit(nc, es)
    es.close()
    return nc


def _host_prep(inputs):
    f = lambda a: np.ascontiguousarray(np.asarray(a, dtype=np.float32))
    x = f(inputs["x"])
    memx = f(inputs["mem"])
    col = lambda v: np.ascontiguousarray(f(v).reshape(16, 128).T)
    gcols = np.concatenate([col(inputs["norm_mix"][0]), col(inputs["norm_cross"][0]),
                            col(inputs["norm_mem"][0]), col(inputs["norm_peer"][0])], axis=1)
    cw = f(inputs["conv_w"])[0]
    cb = f(inputs["conv_b"])[0]
    cp = np.stack([cw[0], cw[1], cw[2], cb], axis=-1)
    convp = np.ascontiguousarray(cp.reshape(8, 128, 4).transpose(1, 0, 2).reshape(128, 32))
    gfin = np.ascontiguousarray(np.broadcast_to(f(inputs["norm_final"])[None, :], (128, D)))
    rel = f(inputs["rel_bias"])[0]
    kl = np.arange(1024)
    q = np.arange(512)
    kc = kl // 64
    b = kl % 64
    qc = q // 64
    a = q % 64
    j = kc[:, None] - qc[None, :]
    valid = (j >= 0) & (j <= 8)
    relpos = (8 - j) * 64 + a[None, :] - b[:, None]
    idx = np.clip(relpos, -128, 128) + 128
    biasT = rel[:, idx]
    biasT = np.where(valid[None], biasT, np.float32(-30000.0)).astype(np.float32)
    biasT = np.ascontiguousarray(biasT.reshape(8, 8, 128, 512))
    sk = f(inputs["sub_keys"])[0]
    skT = np.ascontiguousarray(sk.reshape(16, 128, 128).transpose(2, 0, 1))
    euT = np.ascontiguousarray(f(inputs["expert_u"])[0].reshape(32, 512, 16, 128).transpose(0, 3, 2, 1)).reshape(32, 128, 16 * 512)
    ev = np.ascontiguousarray(f(inputs["expert_v"])[0].reshape(32, 4, 128, D).transpose(0, 2, 1, 3)).reshape(32, 128, 4 * D)
    shared = {
        "w_in": f(inputs["w_in"])[0], "w_att_out": f(inputs["w_att_out"])[0], "w_conv_out": f(inputs["w_conv_out"])[0],
        "w_mix_out": f(inputs["w_mix_out"])[0], "w_cq": f(inputs["w_cq"])[0], "w_ck": f(inputs["w_ck"])[0],
        "w_cv": f(inputs["w_cv"])[0], "w_co": f(inputs["w_co"])[0], "w_pq": f(inputs["w_pq"])[0],
        "gcols": gcols, "convp": convp, "gfin": gfin, "biasT": biasT, "skT": skT, "expert_uT": euT, "expert_v": ev,
        "identf": np.eye(128, dtype=np.float32),
        "iota": np.ascontiguousarray(np.broadcast_to(np.arange(128, dtype=np.float32)[None, :], (128, 128))),
    }
    in_maps = []
    for c in range(8):
        bi, half = c // 2, c % 2
        xh = np.zeros((NTH, D), np.float32)
        if half == 1:
            xh[:] = x[bi, NT - HALO:2 * NT]
        else:
            xh[HALO:] = x[bi, 0:NT]
        m = dict(shared)
        m["xh"] = xh
        m["mem"] = memx[bi]
        m["hv"] = np.full((128, 1), float(half), np.float32)
        in_maps.append(m)
    return in_maps


def kernel(**inputs):
    in_maps = _host_prep(inputs)
    nc = build()
    res = run_bass_kernel_spmd(nc, in_maps, core_ids=list(range(8)))
    out = np.zeros((4, 4096, D), np.float32)
    for c in range(8):
        bi, half = c // 2, c % 2
        out[bi, half * NT:(half + 1) * NT] = res.results[c]["out"]
    return out
```

```python
import os
import numpy as np
from contextlib import ExitStack
import concourse.bass as bass
import concourse.mybir as mybir
from concourse.bass_utils import run_bass_kernel_spmd

F32 = mybir.dt.float32
BF16 = mybir.dt.bfloat16
U32 = mybir.dt.uint32
AF = mybir.ActivationFunctionType
ALU = mybir.AluOpType
AX = mybir.AxisListType

D = 2048
KT = 16
NT = 2048
HALO = 512
NTH = NT + HALO
EPS = 1e-6
ENG = ("sync", "act", "pool", "dve", "pe")
BLK = {"sync": "sync", "act": "scalar", "pool": "gpsimd", "dve": "vector", "pe": "tensor"}
NS = 16


class Buf:
    __slots__ = ("w", "r", "rd")

    def __init__(self):
        self.w = None
        self.r = {}
        self.rd = []


def bufs(n):
    return [Buf() for _ in range(n)]


class Op:
    __slots__ = ("eng", "fn", "dma", "idx", "deps", "needed", "sem", "val", "prev")


class Sched:
    def __init__(self):
        self.ops = []
        self.last = {e: None for e in ENG}
        self.dmas = []
        self.bdeps = {e: [] for e in ENG}

    def add(self, eng, fn, reads=(), writes=(), dma=False):
        op = Op()
        op.eng, op.fn, op.dma, op.idx, op.needed = eng, fn, dma, len(self.ops), False
        op.sem = None
        op.val = 0
        op.prev = 0
        deps = []
        for b in reads:
            if b.w is not None:
                deps.append(b.w)
        for b in writes:
            if b.w is not None:
                deps.append(b.w)
            deps.extend(b.r.values())
            deps.extend(b.rd)
        if self.bdeps[eng]:
            deps.extend(self.bdeps[eng])
            self.bdeps[eng] = []
        best = {}
        dd = {}
        for d in deps:
            if d.dma:
                dd[d.idx] = d
            else:
                if d.eng == "pe" and eng == "pe" and not dma:
                    continue
                if d.eng not in best or best[d.eng].idx < d.idx:
                    best[d.eng] = d
        op.deps = list(best.values()) + list(dd.values())
        for d in op.deps:
            d.needed = True
        for b in reads:
            if dma:
                b.rd.append(op)
            else:
                b.r[eng] = op
        for b in writes:
            b.w = op
            b.r = {}
            b.rd = []
        self.ops.append(op)
        self.last[eng] = op
        if dma:
            self.dmas.append(op)
        return op

    def barrier(self):
        deps = [o for o in self.last.values() if o is not None] + self.dmas
        for e in ENG:
            self.bdeps[e] = list(deps)
        self.dmas = []

    def emit(self, nc, es):
        EPOCH = 2000
        esem = {e: [] for e in ENG}
        dsem = {e: [es.enter_context(nc.semaphore("d_%s_%d" % (e, i))) for i in range(NS)]
                for e in ("sync", "pool", "act")}
        cnt = {e: 0 for e in ENG}
        dcnt = {e: 0 for e in dsem}
        for op in self.ops:
            if op.dma:
                k = dcnt[op.eng]
                dcnt[op.eng] += 1
                op.sem = dsem[op.eng][k % NS]
                op.val = 16 * (k // NS + 1)
                op.prev = 16 * (k // NS)
            elif op.needed:
                c = cnt[op.eng]
                cnt[op.eng] += 1
                ep = c // EPOCH
                while len(esem[op.eng]) <= ep:
                    esem[op.eng].append(es.enter_context(nc.semaphore("s_%s_%d" % (op.eng, len(esem[op.eng])))))
                op.sem = esem[op.eng][ep]
                op.val = c % EPOCH + 1
        block = es.enter_context(nc.Block())
        for e in ENG:
            stream = [op for op in self.ops if op.eng == e]

            def body(eo, stream=stream, e=e):
                waited = {}

                def wait(sem, val):
                    k = id(sem)
                    if waited.get(k, 0) >= val:
                        return
                    eo.wait_ge(sem, val)
                    waited[k] = val

                for op in stream:
                    for d in op.deps:
                        wait(d.sem, d.val)
                    if op.dma and op.prev > 0:
                        wait(op.sem, op.prev)
                    ins = op.fn(eo)
                    if op.dma:
                        ins.then_inc(op.sem, 16)
                    elif op.needed:
                        ins.then_inc(op.sem, 1)
                if e in dsem:
                    n = dcnt[e]
                    for i in range(NS):
                        uses = (n - i + NS - 1) // NS if n > i else 0
                        if uses > 0:
                            wait(dsem[e][i], 16 * uses)

            getattr(block, BLK[e])(body)


def build(stop=99, debug=False):
    nc = bass.Bass("TRN2", target_bir_lowering=False)
    S = Sched()
    es = ExitStack()

    def din(name, shape, dt=F32):
        return nc.dram_tensor(name, list(shape), dt, kind="ExternalInput").ap()

    def dscr(name, shape, dt, out=False):
        if out:
            return nc.dram_tensor(name, list(shape), dt, kind="ExternalOutput").ap()
        return nc.dram_tensor(name, list(shape), dt).ap()

    def sb(name, shape, dt, stack=None):
        return (stack or es).enter_context(nc.sbuf_tensor("sb_" + name, list(shape), dt))

    xh = din("xh", [NTH, D])
    mem = din("mem", [256, D])
    w_in = din("w_in", [D, 10240])
    w_att_out = din("w_att_out", [1024, D])
    w_conv_out = din("w_conv_out", [1024, D])
    w_mix_out = din("w_mix_out", [D, D])
    w_cq = din("w_cq", [D, D])
    w_ck = din("w_ck", [D, D])
    w_cv = din("w_cv", [D, D])
    w_co = din("w_co", [D, D])
    w_pq = din("w_pq", [D, D])
    gcols_d = din("gcols", [128, 64])
    convp_d = din("convp", [128, 32])
    gfin_d = din("gfin", [128, D])
    biasT_d = din("biasT", [8, 8, 128, 512])
    hv_d = din("hv", [128, 1])
    skT_d = din("skT", [128, 16, 128])
    euT = din("expert_uT", [D, 16384])
    ev = din("expert_v", [16384, D])
    identf_d = din("identf", [128, 128])
    iota_d = din("iota", [128, 128])

    out_d = dscr("out", [NT, D], F32, out=True)
    qT_d = dscr("qT_d", [8, 128, NT], BF16)
    kT_d = dscr("kT_d", [8, 128, NTH], BF16)
    v_d = dscr("v_d", [NTH, 1024], BF16)
    sga_d = dscr("sga_d", [16, 128, NT], BF16)
    sgb_d = dscr("sgb_d", [16, 128, NT], BF16)
    x1_d = dscr("x1_d", [NT, D], F32, out=debug)
    x2_d = dscr("x2_d", [NT, D], F32, out=debug)
    ocT_d = dscr("ocT_d", [16, 128, NT], BF16)
    WsD = dscr("WsD", [128, 128, NT], BF16)

    def wview(w):
        return w.rearrange("(kt p) c -> p kt c", p=128)

    identf = sb("identf", [128, 128], F32)
    identb = sb("identb", [128, 128], BF16)
    onesb = sb("onesb", [128, 128], BF16)
    iota = sb("iota", [128, 128], F32)
    gcols = sb("gcols", [128, 64], F32)
    convp = sb("convp", [128, 32], F32)
    hv = sb("hv", [128, 1], F32)
    ss = sb("ss", [128, 32], F32)
    ss2 = sb("ss2", [128, 32], F32)
    rs = sb("rs", [128, 32], F32)
    HO = sb("HO", [128, KT, NT], BF16)
    R = sb("R", [128, 32800], BF16)
    ps = [es.enter_context(nc.psum_tensor("ps%d" % i, [128, 512], F32)) for i in range(8)]
    Bps = bufs(8)
    B_const = Buf()
    B_HO = bufs(16)
    B_ss = bufs(32)
    B_ss2 = bufs(32)
    B_rs = bufs(32)

    def dma(eng, out, in_, reads=(), writes=()):
        return S.add(eng, lambda e: e.dma_start(out=out, in_=in_), reads, writes, dma=True)

    dma("sync", identf[:], identf_d[:, :], writes=[B_const])
    dma("pool", identb[:], identf_d[:, :], writes=[B_const])
    dma("sync", iota[:], iota_d[:, :], writes=[B_const])
    dma("sync", gcols[:], gcols_d[:, :], writes=[B_const])
    dma("sync", convp[:], convp_d[:, :], writes=[B_const])
    dma("sync", hv[:], hv_d[:, :], writes=[B_const])
    S.add("dve", lambda e: e.memset(onesb[:], 1.0), writes=[B_const])
    S.barrier()

    def mm_group(out_ap, pairs, reads, writes):
        def fn(e):
            n = len(pairs)
            ins = None
            for j, (l, r) in enumerate(pairs):
                ins = e.matmul(out_ap, l, r, start=(j == 0), stop=(j == n - 1))
            return ins
        return S.add("pe", fn, reads, writes)

    def norm_phase(stk, ntiles, src_fn, src_bufs_fn, dst_fn, gidx, tag):
        xt = [sb("xt%s%d" % (tag, i), [128, D], F32, stk) for i in range(2)]
        xs = [sb("xs%s%d" % (tag, i), [128, D], BF16, stk) for i in range(2)]
        junk = sb("junk" + tag, [128, D], BF16, stk)
        Bxt, Bxs, Bj = bufs(2), bufs(2), Buf()
        for i in range(ntiles):
            s_ = i % 2
            c = i % 32
            dma("sync", xt[s_][:], src_fn(i), reads=src_bufs_fn(i), writes=[Bxt[s_]])
            S.add("act", lambda e, s_=s_, c=c: e.activation(out=junk[:], in_=xt[s_][:], func=AF.Square,
                                                             accum_out=ss[:, c:c + 1]),
                  reads=[Bxt[s_]], writes=[Bj, B_ss[c]])
            S.add("act", lambda e, c=c: e.activation(out=ss2[:, c:c + 1], in_=ss[:, c:c + 1], func=AF.Sqrt,
                                                     scale=1.0 / D, bias=EPS),
                  reads=[B_ss[c]], writes=[B_ss2[c]])
            S.add("dve", lambda e, c=c: e.reciprocal(out=rs[:, c:c + 1], in_=ss2[:, c:c + 1]),
                  reads=[B_ss2[c]], writes=[B_rs[c]])
            S.add("act", lambda e, s_=s_, c=c: e.activation(out=xs[s_][:], in_=xt[s_][:], func=AF.Copy,
                                                            scale=rs[:, c:c + 1]),
                  reads=[Bxt[s_], B_rs[c]], writes=[Bxs[s_]])
            dst, dbuf = dst_fn(i)
            for half in range(2):
                p = 4 + s_ * 2 + half
                pv = ps[p][:].bitcast(BF16).rearrange("p (a b) -> p a b", a=8)

                def tfn(e, s_=s_, half=half, pv=pv):
                    ins = None
                    for k in range(8):
                        kt = half * 8 + k
                        ins = e.transpose(out=pv[:, k, :], in_=xs[s_][:, kt * 128:(kt + 1) * 128], identity=identb[:])
                    return ins
                S.add("pe", tfn, reads=[Bxs[s_], B_const], writes=[Bps[p]])
                g0 = gidx * 16 + half * 8
                S.add("dve", lambda e, dst=dst, half=half, pv=pv, g0=g0: e.tensor_tensor(
                    out=dst[:, half * 8:(half + 1) * 8, :], in0=pv,
                    in1=gcols[:, g0:g0 + 8].unsqueeze(2).to_broadcast([128, 8, 128]), op=ALU.mult),
                    reads=[Bps[p], B_const], writes=[dbuf])

    B_HH = bufs(4)
    B_qd = [bufs(4) for _ in range(8)]
    B_kd = [bufs(5) for _ in range(8)]
    B_vd = [bufs(2) for _ in range(20)]
    B_sga = [bufs(4) for _ in range(16)]
    B_sgb = [bufs(4) for _ in range(16)]
    B_u = bufs(8)
    B_cv = bufs(8)
    uT = R[:, 0:8 * 2050].rearrange("p (j t) -> p j t", j=8)
    convT = R[:, 16400:16400 + 8 * 2048].rearrange("p (j t) -> p j t", j=8)
    y_attT = R[:, 0:8 * 2048].rearrange("p (j t) -> p j t", j=8)
    B_ya = [bufs(4) for _ in range(8)]

    stkAH = ExitStack()
    HH = sb("HH", [128, KT, HALO], BF16, stkAH)

    def hdst(i):
        if i < 4:
            return HH[:, :, i * 128:(i + 1) * 128], B_HH[i]
        return HO[:, :, (i - 4) * 128:(i - 3) * 128], B_HO[i - 4]

    with ExitStack() as stk:
        norm_phase(stk, 20, lambda i: xh[i * 128:(i + 1) * 128, :], lambda i: [], hdst, 0, "a")
        S.barrier()

    def hgroup(g):
        if g == 0:
            return (lambda kt: HH[:, kt, :]), B_HH
        return (lambda kt, g=g: HO[:, kt, (g - 1) * 512:g * 512]), B_HO[(g - 1) * 4:g * 4]

    psrot = [0]

    def nextps(n=4):
        p = psrot[0] % n
        psrot[0] += 1
        return p

    if stop >= 1:
        with ExitStack() as stk:
            wbuf = [sb("wbufA%d" % i, [128, KT, 512], BF16, stk) for i in range(2)]
            Bwb = bufs(2)
            tmpc = sb("tmpc", [128, NT], F32, stk)
            Btmp = Buf()
            stg = [sb("stgA%d" % i, [128, 512], BF16, stk) for i in range(4)]
            Bstg = bufs(4)
            stgrot = [0]
            w_in_v = wview(w_in)
            order = [6, 7, 10, 11, 8, 9, 0, 1, 2, 3, 4, 5] + list(range(12, 20))
            for n, cg in enumerate(order):
                wb = wbuf[n % 2]
                bw = Bwb[n % 2]
                dma("pool", wb[:], w_in_v[:, :, cg * 512:(cg + 1) * 512], writes=[bw])
                if cg in (4, 5):
                    for tt in range(20):
                        lh, lb = (HH, B_HH[tt]) if tt < 4 else (HO, B_HO[tt - 4])
                        t0 = (tt % 4) * 128 if tt < 4 else (tt - 4) * 128
                        p = nextps()
                        mm_group(ps[p][:], [(lh[:, kt, t0:t0 + 128], wb[:, kt, :]) for kt in range(KT)],
                                 reads=[bw, lb], writes=[Bps[p]])
                        si = stgrot[0] % 4
                        stgrot[0] += 1
                        S.add("act", lambda e, si=si, p=p: e.activation(out=stg[si][:], in_=ps[p][:], func=AF.Copy),
                              reads=[Bps[p]], writes=[Bstg[si]])
                        dma("sync", v_d[tt * 128:(tt + 1) * 128, (cg - 4) * 512:(cg - 3) * 512], stg[si][:],
                            reads=[Bstg[si]], writes=[B_vd[tt][cg - 4]])
                    continue
                for ct in range(4):
                    if cg in (6, 7, 10, 11, 8, 9):
                        j = (cg % 2) * 4 + ct
                        glist = [1, 2, 3, 4] + ([5] if cg in (6, 7, 10, 11) else [])
                    elif cg in (2, 3):
                        glist = [0, 1, 2, 3, 4]
                    else:
                        glist = [1, 2, 3, 4]
                    for g in glist:
                        p = nextps()
                        if g == 5:
                            N = 2
                            rfn, rb = (lambda kt: HH[:, kt, 510:512]), [B_HH[3]]
                        else:
                            N = 512
                            rfn, rb = hgroup(g)
                        mm_group(ps[p][:, 0:N], [(wb[:, kt, ct * 128:(ct + 1) * 128], rfn(kt)) for kt in range(KT)],
                                 reads=[bw] + list(rb), writes=[Bps[p]])
                        if cg in (6, 7):
                            dst = uT[:, j, 0:2] if g == 5 else uT[:, j, 2 + (g - 1) * 512:2 + g * 512]
                            S.add("act", lambda e, dst=dst, p=p, N=N: e.activation(out=dst, in_=ps[p][:, 0:N], func=AF.Copy),
                                  reads=[Bps[p]], writes=[B_u[j]])
                        elif cg in (10, 11):
                            dst = uT[:, j, 0:2] if g == 5 else uT[:, j, 2 + (g - 1) * 512:2 + g * 512]
                            S.add("dve", lambda e, dst=dst, p=p, N=N: e.tensor_tensor(out=dst, in0=ps[p][:, 0:N], in1=dst, op=ALU.mult),
                                  reads=[Bps[p], B_u[j]], writes=[B_u[j]])
                        elif cg in (8, 9):
                            dst = convT[:, j, (g - 1) * 512:g * 512]
                            S.add("dve", lambda e, dst=dst, p=p: e.tensor_tensor(out=dst, in0=ps[p][:], in1=dst, op=ALU.mult),
                                  reads=[Bps[p], B_cv[j]], writes=[B_cv[j]])
                        else:
                            si = stgrot[0] % 4
                            stgrot[0] += 1
                            func = AF.Sigmoid if cg >= 12 else AF.Copy
                            S.add("act", lambda e, si=si, p=p, func=func: e.activation(out=stg[si][:], in_=ps[p][:], func=func),
                                  reads=[Bps[p]], writes=[Bstg[si]])
                            if cg in (0, 1):
                                h = cg * 4 + ct
                                dma("sync", qT_d[h, :, (g - 1) * 512:g * 512], stg[si][:], reads=[Bstg[si]], writes=[B_qd[h][g - 1]])
                            elif cg in (2, 3):
                                h = (cg - 2) * 4 + ct
                                dma("sync", kT_d[h, :, g * 512:(g + 1) * 512], stg[si][:], reads=[Bstg[si]], writes=[B_kd[h][g]])
                            elif cg < 16:
                                c16 = (cg - 12) * 4 + ct
                                dma("sync", sga_d[c16, :, (g - 1) * 512:g * 512], stg[si][:], reads=[Bstg[si]], writes=[B_sga[c16][g - 1]])
                            else:
                                c16 = (cg - 16) * 4 + ct
                                dma("sync", sgb_d[c16, :, (g - 1) * 512:g * 512], stg[si][:], reads=[Bstg[si]], writes=[B_sgb[c16][g - 1]])
                    if cg in (10, 11):
                        w0 = convp[:, j * 4 + 0:j * 4 + 1]
                        w1 = convp[:, j * 4 + 1:j * 4 + 2]
                        w2 = convp[:, j * 4 + 2:j * 4 + 3]
                        bb = convp[:, j * 4 + 3:j * 4 + 4]
                        S.add("dve", lambda e, j=j, w2=w2, bb=bb: e.tensor_scalar(out=tmpc[:], in0=uT[:, j, 2:2050], scalar1=w2, scalar2=bb,
                                                                               op0=ALU.mult, op1=ALU.add),
                              reads=[B_u[j], B_const], writes=[Btmp])
                        S.add("dve", lambda e, j=j, w1=w1: e.scalar_tensor_tensor(out=tmpc[:], in0=uT[:, j, 1:2049], scalar=w1, in1=tmpc[:],
                                                                                   op0=ALU.mult, op1=ALU.add),
                              reads=[B_u[j], Btmp], writes=[Btmp])
                        S.add("dve", lambda e, j=j, w0=w0: e.scalar_tensor_tensor(out=convT[:, j, :], in0=uT[:, j, 0:2048], scalar=w0, in1=tmpc[:],
                                                                                   op0=ALU.mult, op1=ALU.add),
                              reads=[B_u[j], Btmp], writes=[B_cv[j]])
            S.barrier()
    stkAH.close()

    if stop >= 2:
        with ExitStack() as stk:
            qh = [sb("qh%d" % i, [128, NT], BF16, stk) for i in range(2)]
            kh = [sb("kh%d" % i, [128, NTH], BF16, stk) for i in range(2)]
            vh = [sb("vh%d" % i, [128, 20, 128], BF16, stk) for i in range(2)]
            Mh = [sb("Mh%d" % i, [128, 8, 512], BF16, stk) for i in range(2)]
            bst = sb("bst", [128, 8, 512], F32, stk)
            E = [sb("E%d" % i, [128, 512], F32, stk) for i in range(2)]
            PT = [sb("PT%d" % i, [128, 512], BF16, stk) for i in range(3)]
            rz = [sb("rz%d" % i, [128, 512], F32, stk) for i in range(2)]
            Bq, Bk, Bv, BM, Bbst, BE, BPT, Brz = bufs(2), bufs(2), bufs(2), bufs(2), Buf(), bufs(2), bufs(3), bufs(2)
            scale = 128 ** -0.5
            it = 0
            for h in range(8):
                s_ = h % 2
                dma("sync", qh[s_][:], qT_d[h], reads=B_qd[h], writes=[Bq[s_]])
                dma("sync", kh[s_][:], kT_d[h], reads=B_kd[h], writes=[Bk[s_]])
                dma("sync", vh[s_][:], v_d.rearrange("(tt p) c -> p tt c", p=128)[:, :, h * 128:(h + 1) * 128],
                    reads=[B_vd[tt][h // 4] for tt in range(20)], writes=[Bv[s_]])
                dma("sync", bst[:], biasT_d[h].rearrange("i p q -> p i q"), writes=[Bbst])
                S.add("act", lambda e, s_=s_: e.activation(out=Mh[s_][:], in_=bst[:], func=AF.Exp), reads=[Bbst], writes=[BM[s_]])
                for g in range(4):
                    pO = 4 + (g % 2)
                    pZ = 6 + (g % 2)

                    def s_mm(i, itn):
                        pS = itn % 4
                        kc = g * 512 + i * 128
                        S.add("pe", lambda e, pS=pS, s_=s_, kc=kc, g=g: e.matmul(ps[pS][:], kh[s_][:, kc:kc + 128], qh[s_][:, g * 512:(g + 1) * 512],
                                                                                   start=True, stop=True),
                              reads=[Bk[s_], Bq[s_]], writes=[Bps[pS]])
                    s_mm(0, it)
                    s_mm(1, it + 1)
                    for i in range(8):
                        pS = it % 4
                        ei = it % 2
                        S.add("act", lambda e, ei=ei, pS=pS: e.activation(out=E[ei][:], in_=ps[pS][:], func=AF.Exp, scale=scale),
                              reads=[Bps[pS]], writes=[BE[ei]])
                        pi = it % 3
                        if g == 0 and i < 4:
                            S.add("dve", lambda e, pi=pi, ei=ei, s_=s_, i=i: e.scalar_tensor_tensor(
                                out=PT[pi][:], in0=E[ei][:], scalar=hv[:, 0:1], in1=Mh[s_][:, i, :], op0=ALU.mult, op1=ALU.mult),
                                reads=[BE[ei], BM[s_], B_const], writes=[BPT[pi]])
                        else:
                            S.add("dve", lambda e, pi=pi, ei=ei, s_=s_, i=i: e.tensor_tensor(
                                out=PT[pi][:], in0=E[ei][:], in1=Mh[s_][:, i, :], op=ALU.mult),
                                reads=[BE[ei], BM[s_]], writes=[BPT[pi]])
                        if i + 2 < 8:
                            s_mm(i + 2, it + 2)
                        S.add("pe", lambda e, pO=pO, s_=s_, g=g, i=i, pi=pi: e.matmul(ps[pO][:, :], vh[s_][:, g * 4 + i, :], PT[pi][:],
                                                                                       start=(i == 0), stop=(i == 7)),
                              reads=[Bv[s_], BPT[pi]], writes=[Bps[pO]])
                        S.add("pe", lambda e, pZ=pZ, i=i, pi=pi: e.matmul(ps[pZ][:, :], onesb[:], PT[pi][:], start=(i == 0), stop=(i == 7)),
                              reads=[BPT[pi], B_const], writes=[Bps[pZ]])
                        it += 1
                    ri = g % 2
                    S.add("dve", lambda e, ri=ri, pZ=pZ: e.reciprocal(out=rz[ri][:], in_=ps[pZ][:]), reads=[Bps[pZ]], writes=[Brz[ri]])
                    S.add("dve", lambda e, ri=ri, pO=pO, h=h, g=g: e.tensor_tensor(out=y_attT[:, h, g * 512:(g + 1) * 512], in0=ps[pO][:],
                                                                                   in1=rz[ri][:], op=ALU.mult),
                          reads=[Bps[pO], Brz[ri]], writes=[B_ya[h][g]])
            S.barrier()

    B_x1 = [bufs(4) for _ in range(16)]
    if stop >= 3:
        with ExitStack() as stk:
            wa = [sb("wa%d" % i, [128, 8, 512], BF16, stk) for i in range(2)]
            wc = [sb("wc%d" % i, [128, 8, 512], BF16, stk) for i in range(2)]
            sga_t = [sb("sga%d" % i, [128, NT], BF16, stk) for i in range(2)]
            sgb_t = [sb("sgb%d" % i, [128, NT], BF16, stk) for i in range(2)]
            t1 = [sb("t1_%d" % i, [128, 512], F32, stk) for i in range(2)]
            t2 = [sb("t2_%d" % i, [128, 512], F32, stk) for i in range(2)]
            Bwa, Bwc, Bsa, Bsb, Bt1, Bt2 = bufs(2), bufs(2), bufs(2), bufs(2), bufs(2), bufs(2)
            wav = w_att_out.rearrange("(kt p) c -> p kt c", p=128)
            wcv = w_conv_out.rearrange("(kt p) c -> p kt c", p=128)
            it = 0
            for cg in range(4):
                s_ = cg % 2
                dma("pool", wa[s_][:], wav[:, :, cg * 512:(cg + 1) * 512], writes=[Bwa[s_]])
                dma("pool", wc[s_][:], wcv[:, :, cg * 512:(cg + 1) * 512], writes=[Bwc[s_]])
                for ct in range(4):
                    c16 = cg * 4 + ct
                    s2 = c16 % 2
                    dma("sync", sga_t[s2][:], sga_d[c16], reads=B_sga[c16], writes=[Bsa[s2]])
                    dma("sync", sgb_t[s2][:], sgb_d[c16], reads=B_sgb[c16], writes=[Bsb[s2]])
                    for g in range(4):
                        pA = (it % 2) * 2
                        pB = pA + 1
                        ti = it % 2
                        it += 1
                        gs = slice(g * 512, (g + 1) * 512)
                        mm_group(ps[pA][:], [(wa[s_][:, kt, ct * 128:(ct + 1) * 128], y_attT[:, kt, gs]) for kt in range(8)],
                                 reads=[Bwa[s_]] + [B_ya[kt][g] for kt in range(8)], writes=[Bps[pA]])
                        mm_group(ps[pB][:], [(wc[s_][:, kt, ct * 128:(ct + 1) * 128], convT[:, kt, gs]) for kt in range(8)],
                                 reads=[Bwc[s_]] + B_cv, writes=[Bps[pB]])
                        S.add("dve", lambda e, ti=ti, pA=pA, s2=s2, gs=gs: e.tensor_tensor(out=t1[ti][:], in0=ps[pA][:], in1=sga_t[s2][:, gs], op=ALU.mult),
                              reads=[Bps[pA], Bsa[s2]], writes=[Bt1[ti]])
                        S.add("dve", lambda e, ti=ti, pB=pB, s2=s2, gs=gs: e.tensor_tensor(out=t2[ti][:], in0=ps[pB][:], in1=sgb_t[s2][:, gs], op=ALU.mult),
                              reads=[Bps[pB], Bsb[s2]], writes=[Bt2[ti]])
                        S.add("pool", lambda e, ti=ti, c16=c16, gs=gs: e.tensor_tensor(out=HO[:, c16, gs], in0=t1[ti][:], in1=t2[ti][:], op=ALU.add),
                              reads=[Bt1[ti], Bt2[ti]], writes=B_HO[g * 4:(g + 1) * 4])
            S.barrier()
        with ExitStack() as stk:
            RF = R[:, 0:8192].bitcast(F32)
            xp = [RF[:, i * 512:(i + 1) * 512] for i in range(4)]
            x1p = [RF[:, (4 + i) * 512:(5 + i) * 512] for i in range(4)]
            wm = [sb("wm%d" % i, [128, KT, 512], BF16, stk) for i in range(2)]
            Bwm, Bxp, Bx1p = bufs(2), bufs(4), bufs(4)
            wmv = wview(w_mix_out)
            it = 0
            for cg in range(4):
                s_ = cg % 2
                dma("pool", wm[s_][:], wmv[:, :, cg * 512:(cg + 1) * 512], writes=[Bwm[s_]])
                def xload(cg_, tt_, it_):
                    dma("sync", xp[it_ % 4][:], xh[HALO + tt_ * 128:HALO + (tt_ + 1) * 128, cg_ * 512:(cg_ + 1) * 512], writes=[Bxp[it_ % 4]])
                if cg == 0:
                    xload(0, 0, 0)
                    xload(0, 1, 1)
                for tt in range(16):
                    xi = it % 4
                    p = it % 4
                    nx = cg * 16 + tt + 2
                    if nx < 64:
                        xload(nx // 16, nx % 16, it + 2)
                    it += 1
                    mm_group(ps[p][:], [(HO[:, kt, tt * 128:(tt + 1) * 128], wm[s_][:, kt, :]) for kt in range(KT)],
                             reads=[Bwm[s_], B_HO[tt]], writes=[Bps[p]])
                    S.add("dve", lambda e, xi=xi, p=p: e.tensor_tensor(out=x1p[xi][:], in0=ps[p][:], in1=xp[xi][:], op=ALU.add),
                          reads=[Bps[p], Bxp[xi]], writes=[Bx1p[xi]])
                    dma("sync", x1_d[tt * 128:(tt + 1) * 128, cg * 512:(cg + 1) * 512], x1p[xi][:], reads=[Bx1p[xi]], writes=[B_x1[tt][cg]])
            S.barrier()

    B_x2 = [bufs(4) for _ in range(16)]
    B_oc = [bufs(4) for _ in range(16)]
    if stop >= 4:
        stkB = ExitStack()
        memT = sb("memT", [128, KT, 256], BF16, stkB)
        kmT = sb("kmT", [128, 16, 256], BF16, stkB)
        vm = sb("vm", [128, 2, D], BF16, stkB)
        BmemT, BkmT, Bvm = bufs(2), Buf(), Buf()
        with ExitStack() as stk:
            norm_phase(stk, 2, lambda i: mem[i * 128:(i + 1) * 128, :], lambda i: [],
                       lambda i: (memT[:, :, i * 128:(i + 1) * 128], BmemT[i]), 2, "m")
            S.barrier()
        with ExitStack() as stk:
            wbuf = [sb("wbufB%d" % i, [128, KT, 512], BF16, stk) for i in range(2)]
            Bwb = bufs(2)
            n = 0
            import os
            for wsrc, kind in (() if "B0w" in os.environ.get("KSKIP", "") else ((w_ck, 0), (w_cv, 1))):
                wv_ = wview(wsrc)
                for cg in range(4):
                    wb, bw = wbuf[n % 2], Bwb[n % 2]
                    n += 1
                    dma("pool", wb[:], wv_[:, :, cg * 512:(cg + 1) * 512], writes=[bw])
                    if kind == 0:
                        for ct in range(4):
                            p = nextps()
                            mm_group(ps[p][:, 0:256], [(wb[:, kt, ct * 128:(ct + 1) * 128], memT[:, kt, :]) for kt in range(KT)],
                                     reads=[bw] + BmemT, writes=[Bps[p]])
                            S.add("act", lambda e, p=p, c16=cg * 4 + ct: e.activation(out=kmT[:, c16, :], in_=ps[p][:, 0:256], func=AF.Copy),
                                  reads=[Bps[p]], writes=[BkmT])
                    else:
                        for mt in range(2):
                            p = nextps()
                            mm_group(ps[p][:], [(memT[:, kt, mt * 128:(mt + 1) * 128], wb[:, kt, :]) for kt in range(KT)],
                                     reads=[bw] + BmemT, writes=[Bps[p]])
                            S.add("act", lambda e, p=p, mt=mt, cg=cg: e.activation(out=vm[:, mt, cg * 512:(cg + 1) * 512], in_=ps[p][:], func=AF.Copy),
                                  reads=[Bps[p]], writes=[Bvm])
            S.barrier()
        with ExitStack() as stk:
            import os
            norm_phase(stk, 0 if "B1" in os.environ.get("KSKIP", "") else 16, lambda i: x1_d[i * 128:(i + 1) * 128, :], lambda i: B_x1[i],
                       lambda i: (HO[:, :, i * 128:(i + 1) * 128], B_HO[i]), 1, "b")
            S.barrier()
        import os
        SKIP = os.environ.get("KSKIP", "")
        with ExitStack() as stk:
            if "B2" in SKIP:
                raise_skip = True
            wbuf = [sb("wbufQ%d" % i, [128, KT, 512], BF16, stk) for i in range(2)]
            Bwb = bufs(2)
            qcT = R[:, 0:4 * NT].rearrange("p (c t) -> p c t", c=4)
            Bqc = [bufs(4) for _ in range(4)]
            Ec = [sb("Ec%d" % i, [128, 512], BF16, stk) for i in range(4)]
            BEc = bufs(4)
            rzc = [sb("rzc%d" % i, [128, 512], F32, stk) for i in range(2)]
            Brzc = bufs(2)
            stgB2 = [sb("stgB%d" % i, [128, 512], BF16, stk) for i in range(4)]
            BstgB2 = bufs(4)
            wqv = wview(w_cq)
            scaleB = 512 ** -0.5
            it = 0
            si_rot = 0
            for hh in range(0 if "B2" in SKIP else 4):
                wb, bw = wbuf[hh % 2], Bwb[hh % 2]
                dma("pool", wb[:], wqv[:, :, hh * 512:(hh + 1) * 512], writes=[bw])
                for ct in range(4):
                    for g in range(4):
                        p = nextps(2)
                        mm_group(ps[p][:], [(wb[:, kt, ct * 128:(ct + 1) * 128], HO[:, kt, g * 512:(g + 1) * 512]) for kt in range(KT)],
                                 reads=[bw] + B_HO[g * 4:(g + 1) * 4], writes=[Bps[p]])
                        S.add("act", lambda e, p=p, ct=ct, g=g: e.activation(out=qcT[:, ct, g * 512:(g + 1) * 512], in_=ps[p][:], func=AF.Copy),
                              reads=[Bps[p]], writes=[Bqc[ct][g]])
                for g in range(4):
                    gs = slice(g * 512, (g + 1) * 512)
                    eis = []
                    for mt in range(2):
                        p = 2 + (it % 2)
                        ei = it % 4
                        it += 1
                        eis.append(ei)
                        mm_group(ps[p][:], [(kmT[:, hh * 4 + ct, mt * 128:(mt + 1) * 128], qcT[:, ct, gs]) for ct in range(4)],
                                 reads=[BkmT] + [Bqc[ct][g] for ct in range(4)], writes=[Bps[p]])
                        S.add("act", lambda e, p=p, ei=ei: e.activation(out=Ec[ei][:], in_=ps[p][:], func=AF.Exp, scale=scaleB),
                              reads=[Bps[p]], writes=[BEc[ei]])
                    pZ = 4 + (g % 2)
                    ri = g % 2
                    mm_group(ps[pZ][:], [(onesb[:], Ec[eis[mt]][:]) for mt in range(2)], reads=[BEc[e_] for e_ in eis] + [B_const], writes=[Bps[pZ]])
                    S.add("dve", lambda e, ri=ri, pZ=pZ: e.reciprocal(out=rzc[ri][:], in_=ps[pZ][:]), reads=[Bps[pZ]], writes=[Brzc[ri]])
                    for ct in range(4):
                        c16 = hh * 4 + ct
                        pO = 6 + (ct % 2)
                        mm_group(ps[pO][:], [(vm[:, mt, c16 * 128:(c16 + 1) * 128], Ec[eis[mt]][:]) for mt in range(2)],
                                 reads=[Bvm] + [BEc[e_] for e_ in eis], writes=[Bps[pO]])
                        si = si_rot % 4
                        si_rot += 1
                        S.add("dve", lambda e, si=si, pO=pO, ri=ri: e.tensor_tensor(out=stgB2[si][:], in0=ps[pO][:], in1=rzc[ri][:], op=ALU.mult),
                              reads=[Bps[pO], Brzc[ri]], writes=[BstgB2[si]])
                        dma("sync", ocT_d[c16, :, gs], stgB2[si][:], reads=[BstgB2[si]], writes=[B_oc[c16][g]])
            S.barrier()
        stkB.close()
        with ExitStack() as stk:
            RF = R[:, 16384:24576].bitcast(F32)
            xpB3 = [RF[:, i * 512:(i + 1) * 512] for i in range(4)]
            x2p = [RF[:, (4 + i) * 512:(5 + i) * 512] for i in range(4)]
            wbuf = [sb("wbufO%d" % i, [128, KT, 512], BF16, stk) for i in range(2)]
            Bwb = bufs(2)
            ocg = [sb("ocg%d" % i, [128, KT, 512], BF16, stk) for i in range(2)]
            Bocg = bufs(2)
            BxpB3, Bx2p = bufs(4), bufs(4)
            wov = wview(w_co)
            it = 0
            n = 0
            for cg in range(0 if "B3" in SKIP else 4):
                wb, bw = wbuf[cg % 2], Bwb[cg % 2]
                dma("pool", wb[:], wov[:, :, cg * 512:(cg + 1) * 512], writes=[bw])
                def ocload(n_):
                    g_ = n_ % 4
                    dma("sync", ocg[n_ % 2][:], ocT_d.rearrange("c p t -> p c t")[:, :, g_ * 512:(g_ + 1) * 512],
                        reads=[B_oc[c][g_] for c in range(16)], writes=[Bocg[n_ % 2]])

                def xloadB(it_):
                    cg_, tt_ = it_ // 16, it_ % 16
                    dma("sync", xpB3[it_ % 4][:], x1_d[tt_ * 128:(tt_ + 1) * 128, cg_ * 512:(cg_ + 1) * 512], reads=[B_x1[tt_][cg_]], writes=[BxpB3[it_ % 4]])
                if cg == 0:
                    ocload(0)
                    xloadB(0)
                    xloadB(1)
                for g in range(4):
                    oi = n % 2
                    n += 1
                    if n < 16:
                        ocload(n)
                    for t4 in range(4):
                        tt = g * 4 + t4
                        xi = it % 4
                        p = it % 4
                        if it + 2 < 64:
                            xloadB(it + 2)
                        it += 1
                        mm_group(ps[p][:], [(ocg[oi][:, kt, t4 * 128:(t4 + 1) * 128], wb[:, kt, :]) for kt in range(KT)],
                                 reads=[bw, Bocg[oi]], writes=[Bps[p]])
                        S.add("dve", lambda e, xi=xi, p=p: e.tensor_tensor(out=x2p[xi][:], in0=ps[p][:], in1=xpB3[xi][:], op=ALU.add),
                              reads=[Bps[p], BxpB3[xi]], writes=[Bx2p[xi]])
                        dma("sync", x2_d[tt * 128:(tt + 1) * 128, cg * 512:(cg + 1) * 512], x2p[xi][:], reads=[Bx2p[xi]], writes=[B_x2[tt][cg]])
            S.barrier()

    if stop >= 5:
        with ExitStack() as stk:
            norm_phase(stk, 16, lambda i: x2_d[i * 128:(i + 1) * 128, :], lambda i: B_x2[i],
                       lambda i: (HO[:, :, i * 128:(i + 1) * 128], B_HO[i]), 3, "c")
            S.barrier()
        B_Ws = bufs(16)
        with ExitStack() as stk:
            wpq = sb("wpq", [128, KT, 512], BF16, stk)
            Bwpq = Buf()
            skT = sb("skT", [128, 16, 128], BF16, stk)
            BskT = Buf()
            qpT = sb("qpT", [128, 16, 512], BF16, stk)
            Bqp = bufs(16)
            s_sb = sb("s_sb", [128, 16, 128], F32, stk)
            Bsh = bufs(16)
            mx = sb("mx", [128, 16, 16], F32, stk)
            ix = sb("ix", [128, 16, 16], U32, stk)
            ixf = sb("ixf", [128, 16, 16], F32, stk)
            Bmx, Bix, Bixf = bufs(16), bufs(16), Buf()
            cand = sb("cand", [128, 8, 256], F32, stk)
            Bch = bufs(8)
            top = sb("top", [128, 8, 16], F32, stk)
            pos = sb("pos", [128, 8, 16], U32, stk)
            Btop, Bpos = bufs(8), bufs(8)
            ir = sb("ir", [128, 128], U32, stk)
            jr = sb("jr", [128, 128], U32, stk)
            irf = sb("irf", [128, 8, 16], F32, stk)
            jrf = sb("jrf", [128, 8, 16], F32, stk)
            oh = sb("oh", [128, 8, 16, 16], F32, stk)
            Boh = Buf()
            negm = sb("negm", [128, 8], F32, stk)
            Z = sb("Z", [128, 8], F32, stk)
            rZ = sb("rZ", [128, 8], F32, stk)
            et = sb("et", [128, 8, 16], F32, stk)
            Bmisc = bufs(8)
            tok3 = sb("tok3", [128, 3, 128], F32, stk)
            Btok3 = bufs(3)
            slot3s = [sb("slot3_%d" % i, [128, 3, 128], F32, stk) for i in range(2)]
            Bslot3s = bufs(2)
            P2s = [R[:, i * 4096:(i + 1) * 4096].rearrange("p (t k) -> p t k", t=32) for i in range(2)]
            P1s = [R[:, 8192 + i * 4096:8192 + (i + 1) * 4096].rearrange("p (t k) -> p t k", t=32) for i in range(2)]
            BP2s, BP1s = bufs(2), bufs(2)
            pq_rot = [0]
            wrot = [0]
            WT = R[:, 16384:32768].rearrange("p (k t) -> p k t", k=128)
            BWT = Buf()
            dma("pool", skT[:], skT_d[:, :, :], writes=[BskT])
            wpv = wview(w_pq)
            iota_b = iota[:, 0:16]
            def projection(g):
                gsl = slice(g * 512, (g + 1) * 512)
                for cg in range(4):
                    dma("pool", wpq[:], wpv[:, :, cg * 512:(cg + 1) * 512], writes=[Bwpq])
                    for ct in range(4):
                        p = nextps(2)
                        hp = cg * 4 + ct
                        mm_group(ps[p][:], [(wpq[:, kt, ct * 128:(ct + 1) * 128], HO[:, kt, gsl]) for kt in range(KT)],
                                 reads=[Bwpq] + B_HO[g * 4:(g + 1) * 4], writes=[Bps[p]])
                        S.add("act", lambda e, p=p, hp=hp: e.activation(out=qpT[:, hp, :], in_=ps[p][:], func=AF.Copy),
                              reads=[Bps[p]], writes=[Bqp[hp]])
            def stageA(g, t4, sl):
                tt = g * 4 + t4
                tsl = slice(t4 * 128, (t4 + 1) * 128)
                for b in range(4):
                    p = 2 + (b % 2)

                    def sfn(e, p=p, b=b, tsl=tsl):
                        ins = None
                        for q in range(4):
                            hp = b * 4 + q
                            ins = e.matmul(ps[p][:, q * 128:(q + 1) * 128], qpT[:, hp, tsl], skT[:, hp, :], start=True, stop=True)
                        return ins
                    S.add("pe", sfn, reads=Bqp[b * 4:(b + 1) * 4] + [BskT], writes=[Bps[p]])
                    S.add("act", lambda e, p=p, b=b: e.activation(out=s_sb[:, b * 4:(b + 1) * 4, :].rearrange("p a b -> p (a b)"),
                                                                  in_=ps[p][:], func=AF.Copy),
                          reads=[Bps[p]], writes=Bsh[b * 4:(b + 1) * 4])
                for hp in range(16):
                    S.add("dve", lambda e, hp=hp: e.max(out=mx[:, hp, 0:8], in_=s_sb[:, hp, :]), reads=[Bsh[hp]], writes=[Bmx[hp]])
                for hp in range(16):
                    S.add("dve", lambda e, hp=hp: e.max_index(out=ix[:, hp, 0:8], in_max=mx[:, hp, 0:8], in_values=s_sb[:, hp, :]),
                          reads=[Bsh[hp], Bmx[hp]], writes=[Bix[hp]])
                for hp in range(16):
                    S.add("dve", lambda e, hp=hp: e.match_replace(out=s_sb[:, hp, :], in_to_replace=mx[:, hp, 0:8], in_values=s_sb[:, hp, :],
                                                                  imm_value=-1e30),
                          reads=[Bmx[hp]], writes=[Bsh[hp]])
                for hp in range(16):
                    S.add("dve", lambda e, hp=hp: e.max(out=mx[:, hp, 8:16], in_=s_sb[:, hp, :]), reads=[Bsh[hp]], writes=[Bmx[hp]])
                for hp in range(16):
                    S.add("dve", lambda e, hp=hp: e.max_index(out=ix[:, hp, 8:16], in_max=mx[:, hp, 8:16], in_values=s_sb[:, hp, :]),
                          reads=[Bsh[hp], Bmx[hp]], writes=[Bix[hp]])
                S.add("dve", lambda e: e.tensor_copy(out=ixf[:], in_=ix[:]), reads=Bix, writes=[Bixf])
                mxv = mx[:].rearrange("p (h two) k -> p h two k", two=2)
                for hh2 in range(2):
                    S.add("dve", lambda e, mxv=mxv, hh2=hh2: e.tensor_tensor(
                        out=cand[:, hh2 * 4:(hh2 + 1) * 4, :].rearrange("p h (i j) -> p h i j", i=16),
                        in0=mxv[:, hh2 * 4:(hh2 + 1) * 4, 0, :].unsqueeze(3).to_broadcast([128, 4, 16, 16]),
                        in1=mxv[:, hh2 * 4:(hh2 + 1) * 4, 1, :].unsqueeze(2).to_broadcast([128, 4, 16, 16]), op=ALU.add),
                        reads=Bmx, writes=Bch[hh2 * 4:(hh2 + 1) * 4])
                for h in range(8):
                    S.add("dve", lambda e, h=h: e.max(out=top[:, h, 0:8], in_=cand[:, h, :]), reads=[Bch[h]], writes=[Btop[h]])
                for h in range(8):
                    S.add("dve", lambda e, h=h: e.max_index(out=pos[:, h, 0:8], in_max=top[:, h, 0:8], in_values=cand[:, h, :]),
                          reads=[Bch[h], Btop[h]], writes=[Bpos[h]])
                for h in range(8):
                    S.add("dve", lambda e, h=h: e.match_replace(out=cand[:, h, :], in_to_replace=top[:, h, 0:8], in_values=cand[:, h, :],
                                                                imm_value=-1e30),
                          reads=[Btop[h]], writes=[Bch[h]])
                for h in range(8):
                    S.add("dve", lambda e, h=h: e.max(out=top[:, h, 8:16], in_=cand[:, h, :]), reads=[Bch[h]], writes=[Btop[h]])
                for h in range(8):
                    S.add("dve", lambda e, h=h: e.max_index(out=pos[:, h, 8:16], in_max=top[:, h, 8:16], in_values=cand[:, h, :]),
                          reads=[Bch[h], Btop[h]], writes=[Bpos[h]])
                posf = pos[:].rearrange("p h r -> p (h r)")
                S.add("dve", lambda e, posf=posf: e.tensor_single_scalar(out=ir[:], in_=posf, scalar=4, op=ALU.logical_shift_right),
                      reads=Bpos, writes=[Bmisc[0]])
                S.add("dve", lambda e, posf=posf: e.tensor_single_scalar(out=jr[:], in_=posf, scalar=15, op=ALU.bitwise_and),
                      reads=Bpos, writes=[Bmisc[1]])
                S.add("dve", lambda e: e.tensor_copy(out=irf[:].rearrange("p h r -> p (h r)"), in_=ir[:]), reads=[Bmisc[0]], writes=[Bmisc[2]])
                S.add("dve", lambda e: e.tensor_copy(out=jrf[:].rearrange("p h r -> p (h r)"), in_=jr[:]), reads=[Bmisc[1]], writes=[Bmisc[3]])
                ixv = ixf[:].rearrange("p (h two) k -> p h two k", two=2)
                for which, rf, bm in ((0, irf, Bmisc[2]), (1, jrf, Bmisc[3])):
                    S.add("dve", lambda e, rf=rf: e.tensor_tensor(
                        out=oh[:], in0=rf[:].unsqueeze(3).to_broadcast([128, 8, 16, 16]),
                        in1=iota_b.unsqueeze(1).unsqueeze(1).to_broadcast([128, 8, 16, 16]), op=ALU.is_equal),
                        reads=[bm, B_const], writes=[Boh])
                    S.add("dve", lambda e, which=which, ixv=ixv: e.tensor_tensor(
                        out=oh[:], in0=oh[:], in1=ixv[:, :, which, :].unsqueeze(2).to_broadcast([128, 8, 16, 16]), op=ALU.mult),
                        reads=[Boh, Bixf], writes=[Boh])
                    S.add("dve", lambda e, which=which: e.tensor_reduce(
                        out=tok3[:, which, :], in_=oh[:].rearrange("p h r i -> p (h r) i"), axis=AX.X, op=ALU.add),
                        reads=[Boh], writes=[Btok3[which]])
                S.add("dve", lambda e: e.tensor_scalar(out=negm[:], in0=top[:, :, 0], scalar1=-1.0, scalar2=None, op0=ALU.mult),
                      reads=Btop, writes=[Bmisc[4]])
                for h in range(8):
                    S.add("act", lambda e, h=h: e.activation(out=et[:, h, :], in_=top[:, h, :], func=AF.Exp, bias=negm[:, h:h + 1],
                                                             accum_out=Z[:, h:h + 1]),
                          reads=[Btop[h], Bmisc[4]], writes=[Bmisc[5]])
                S.add("dve", lambda e: e.reciprocal(out=rZ[:], in_=Z[:]), reads=[Bmisc[5]], writes=[Bmisc[6]])
                S.add("dve", lambda e: e.tensor_tensor(out=tok3[:, 2, :].rearrange("p (h r) -> p h r", h=8), in0=et[:],
                                                       in1=rZ[:].unsqueeze(2).to_broadcast([128, 8, 16]), op=ALU.mult),
                      reads=[Bmisc[5], Bmisc[6]], writes=[Btok3[2]])
                p = 4

                def trfn(e, p=p):
                    ins = None
                    for w_ in range(3):
                        ins = e.transpose(out=ps[p][:, w_ * 128:(w_ + 1) * 128], in_=tok3[:, w_, :], identity=identf[:])
                    return ins
                S.add("pe", trfn, reads=Btok3 + [B_const], writes=[Bps[p]])
                S.add("act", lambda e, p=p: e.activation(out=slot3s[sl][:].rearrange("p a b -> p (a b)"), in_=ps[p][:, 0:384], func=AF.Copy),
                      reads=[Bps[p]], writes=[Bslot3s[sl]])
            def stageB(tt, sl):
                for qt in range(4):
                    hs = slice(qt * 32, (qt + 1) * 32)
                    pb = pq_rot[0] % 2
                    pq_rot[0] += 1
                    P2 = P2s[pb]
                    P1 = P1s[pb]
                    S.add("dve", lambda e, hs=hs, P2=P2: e.tensor_tensor(
                        out=P2, in0=iota[:].unsqueeze(1).to_broadcast([128, 32, 128]),
                        in1=slot3s[sl][:, 1, hs].unsqueeze(2).to_broadcast([128, 32, 128]), op=ALU.is_equal),
                        reads=[Bslot3s[sl], B_const], writes=[BP2s[pb]])
                    S.add("dve", lambda e, hs=hs, P1=P1: e.tensor_tensor(
                        out=P1, in0=iota[:].unsqueeze(1).to_broadcast([128, 32, 128]),
                        in1=slot3s[sl][:, 0, hs].unsqueeze(2).to_broadcast([128, 32, 128]), op=ALU.is_equal),
                        reads=[Bslot3s[sl], B_const], writes=[BP1s[pb]])
                    S.add("pool", lambda e, hs=hs, P1=P1: e.tensor_tensor(
                        out=P1, in0=P1, in1=slot3s[sl][:, 2, hs].unsqueeze(2).to_broadcast([128, 32, 128]), op=ALU.mult),
                        reads=[Bslot3s[sl], BP1s[pb]], writes=[BP1s[pb]])
                    for q4 in range(8):
                        p = 5 + (wrot[0] % 3)
                        wrot[0] += 1

                        def wfn(e, p=p, q4=q4, P1=P1, P2=P2):
                            ins = None
                            pv = ps[p][:].rearrange("p (k t) -> p k t", t=4)
                            for q in range(4):
                                tl = q4 * 4 + q
                                ins = e.matmul(pv[:, :, q], P2[:, tl, :], P1[:, tl, :], start=True, stop=True)
                            return ins
                        S.add("pe", wfn, reads=[BP1s[pb], BP2s[pb]], writes=[Bps[p]])
                        t0 = qt * 32 + q4 * 4
                        S.add("act", lambda e, p=p, t0=t0: e.activation(
                            out=WT[:, :, t0:t0 + 4],
                            in_=ps[p][:].rearrange("p (k t) -> p k t", t=4), func=AF.Copy),
                            reads=[Bps[p]], writes=[BWT])
                Wv = WsD.rearrange("k1 k2 t -> k2 k1 t")
                for q8 in range(8):
                    dma("sync", Wv[:, q8 * 16:(q8 + 1) * 16, tt * 128:(tt + 1) * 128], WT[:, q8 * 16:(q8 + 1) * 16, :],
                        reads=[BWT], writes=[B_Ws[tt]])
            tiles = [(g, t4) for g in range(int(os.environ.get("KC1", "4"))) for t4 in range(4)]
            for n_, (g, t4) in enumerate(tiles):
                if t4 == 0:
                    projection(g)
                stageA(g, t4, n_ % 2)
                if n_ >= 1:
                    stageB(tiles[n_ - 1][0] * 4 + tiles[n_ - 1][1], (n_ - 1) % 2)
            if tiles:
                stageB(tiles[-1][0] * 4 + tiles[-1][1], (len(tiles) - 1) % 2)
            S.barrier()
        with ExitStack() as stk:
            ucT = [R[:, i * 8192:(i + 1) * 8192].rearrange("p (kt c) -> p kt c", kt=KT) for i in range(2)]
            vc = [R[:, 16384 + i * 8192:16384 + (i + 1) * 8192].rearrange("p (k d) -> p k d", k=4) for i in range(2)]
            Buc, Bvc = bufs(2), bufs(2)
            oacc = sb("oacc", [128, 4, D], F32, stk)
            Boacc = [bufs(4) for _ in range(4)]
            wch = [sb("wch%d" % i, [128, 4, 512], BF16, stk) for i in range(2)]
            Bwch = bufs(2)
            ge = [sb("ge%d" % i, [128, 512], F32, stk) for i in range(2)]
            Bge = bufs(2)
            actd = [sb("actd%d" % i, [128, 4, 512], BF16, stk) for i in range(2)]
            Bactd = bufs(2)
            xq = [sb("xq%d" % i, [128, 512], F32, stk) for i in range(2)]
            gq = [sb("gq%d" % i, [128, 512], F32, stk) for i in range(2)]
            Bxq, Bgq = bufs(2), bufs(2)
            junk = sb("junkF", [128, D], BF16, stk)
            Bj = Buf()
            euv = euT.rearrange("(kt p) e -> p kt e", p=128)
            evv = ev.rearrange("(k p) d -> p k d", p=128)
            arot = [0]
            orot = [0]
            fi = 0
            for g in range(int(os.environ.get("KC4", "4"))):
                gsl = slice(g * 512, (g + 1) * 512)
                def loads(c):
                    s_ = c % 2
                    dma("pool", ucT[s_], euv[:, :, c * 512:(c + 1) * 512], writes=[Buc[s_]])
                    dma("pool", vc[s_], evv[:, c * 4:(c + 1) * 4, :], writes=[Bvc[s_]])
                    dma("sync", wch[s_][:], WsD[c * 4:(c + 1) * 4, :, gsl].rearrange("k p t -> p k t"),
                        reads=B_Ws[g * 4:(g + 1) * 4], writes=[Bwch[s_]])

                def a_group(c, k):
                    s_ = c % 2
                    p = arot[0] % 2
                    gi = arot[0] % 2
                    arot[0] += 1
                    mm_group(ps[p][:], [(ucT[s_][:, kt, k * 128:(k + 1) * 128], HO[:, kt, gsl]) for kt in range(KT)],
                             reads=[Buc[s_]] + B_HO[g * 4:(g + 1) * 4], writes=[Bps[p]])
                    S.add("act", lambda e, p=p, gi=gi: e.activation(out=ge[gi][:], in_=ps[p][:], func=AF.Gelu),
                          reads=[Bps[p]], writes=[Bge[gi]])
                    S.add("dve", lambda e, gi=gi, s_=s_, k=k: e.tensor_tensor(out=actd[s_][:, k, :], in0=ge[gi][:], in1=wch[s_][:, k, :], op=ALU.mult),
                          reads=[Bge[gi], Bwch[s_]], writes=[Bactd[s_]])

                def o_group(c, j):
                    s_ = c % 2
                    t4, dg = j // 4, j % 4
                    p = 2 + (orot[0] % 6)
                    orot[0] += 1
                    dsl = slice(dg * 512, (dg + 1) * 512)
                    mm_group(ps[p][:], [(actd[s_][:, k, t4 * 128:(t4 + 1) * 128], vc[s_][:, k, dsl]) for k in range(4)],
                             reads=[Bactd[s_], Bvc[s_]], writes=[Bps[p]])
                    if c == 0:
                        S.add("dve", lambda e, p=p, t4=t4, dsl=dsl: e.tensor_copy(out=oacc[:, t4, dsl], in_=ps[p][:]),
                              reads=[Bps[p]], writes=[Boacc[t4][dg]])
                    else:
                        S.add("dve", lambda e, p=p, t4=t4, dsl=dsl: e.tensor_tensor(out=oacc[:, t4, dsl], in0=ps[p][:], in1=oacc[:, t4, dsl], op=ALU.add),
                              reads=[Bps[p], Boacc[t4][dg]], writes=[Boacc[t4][dg]])

                loads(0)
                for k in range(4):
                    a_group(0, k)
                for c in range(32):
                    if c + 1 < 32:
                        loads(c + 1)
                    for j in range(16):
                        if j % 4 == 0 and c + 1 < 32:
                            a_group(c + 1, j // 4)
                        o_group(c, j)
                for t4 in range(4):
                    tt = g * 4 + t4
                    c = 16 + (tt % 16)
                    for dg in range(4):
                        dsl = slice(dg * 512, (dg + 1) * 512)
                        xi = fi % 2
                        fi += 1
                        dma("sync", xq[xi][:], x2_d[tt * 128:(tt + 1) * 128, dsl], reads=[B_x2[tt][dg]], writes=[Bxq[xi]])
                        S.add("dve", lambda e, t4=t4, dsl=dsl, xi=xi: e.tensor_tensor(out=oacc[:, t4, dsl], in0=oacc[:, t4, dsl], in1=xq[xi][:], op=ALU.add),
                              reads=[Bxq[xi], Boacc[t4][dg]], writes=[Boacc[t4][dg]])
                    S.add("act", lambda e, t4=t4, c=c: e.activation(out=junk[:], in_=oacc[:, t4, :], func=AF.Square, accum_out=ss[:, c:c + 1]),
                          reads=Boacc[t4], writes=[Bj, B_ss[c]])
                    S.add("act", lambda e, c=c: e.activation(out=ss2[:, c:c + 1], in_=ss[:, c:c + 1], func=AF.Sqrt, scale=1.0 / D, bias=EPS),
                          reads=[B_ss[c]], writes=[B_ss2[c]])
                    S.add("dve", lambda e, c=c: e.reciprocal(out=rs[:, c:c + 1], in_=ss2[:, c:c + 1]), reads=[B_ss2[c]], writes=[B_rs[c]])
                    for dg in range(4):
                        dsl = slice(dg * 512, (dg + 1) * 512)
                        xi = fi % 2
                        fi += 1
                        dma("sync", gq[xi][:], gfin_d[:, dsl], writes=[Bgq[xi]])
                        S.add("dve", lambda e, t4=t4, dsl=dsl, xi=xi, c=c: e.scalar_tensor_tensor(
                            out=oacc[:, t4, dsl], in0=oacc[:, t4, dsl], scalar=rs[:, c:c + 1], in1=gq[xi][:], op0=ALU.mult, op1=ALU.mult),
                            reads=[Bgq[xi], Boacc[t4][dg], B_rs[c]], writes=[Boacc[t4][dg]])
                    dma("sync", out_d[tt * 128:(tt + 1) * 128, :], oacc[:, t4, :], reads=Boacc[t4], writes=[])
            S.barrier()

    S.emit(nc, es)
    es.close()
    return nc


def _host_prep(inputs):
    f = lambda a: np.ascontiguousarray(np.asarray(a, dtype=np.float32))
    x = f(inputs["x"])
    memx = f(inputs["mem"])
    col = lambda v: np.ascontiguousarray(f(v).reshape(16, 128).T)
    gcols = np.concatenate([col(inputs["norm_mix"][0]), col(inputs["norm_cross"][0]),
                            col(inputs["norm_mem"][0]), col(inputs["norm_peer"][0])], axis=1)
    cw = f(inputs["conv_w"])[0]
    cb = f(inputs["conv_b"])[0]
    cp = np.stack([cw[0], cw[1], cw[2], cb], axis=-1)
    convp = np.ascontiguousarray(cp.reshape(8, 128, 4).transpose(1, 0, 2).reshape(128, 32))
    gfin = np.ascontiguousarray(np.broadcast_to(f(inputs["norm_final"])[None, :], (128, D)))
    rel = f(inputs["rel_bias"])[0]
    kl = np.arange(1024)
    q = np.arange(512)
    kc = kl // 64
    b = kl % 64
    qc = q // 64
    a = q % 64
    j = kc[:, None] - qc[None, :]
    valid = (j >= 0) & (j <= 8)
    relpos = (8 - j) * 64 + a[None, :] - b[:, None]
    idx = np.clip(relpos, -128, 128) + 128
    biasT = rel[:, idx]
    biasT = np.where(valid[None], biasT, np.float32(-30000.0)).astype(np.float32)
    biasT = np.ascontiguousarray(biasT.reshape(8, 8, 128, 512))
    sk = f(inputs["sub_keys"])[0]
    skT = np.ascontiguousarray(sk.reshape(16, 128, 128).transpose(2, 0, 1))
    euT = np.ascontiguousarray(f(inputs["expert_u"])[0].T)
    ev = f(inputs["expert_v"])[0]
    shared = {
        "w_in": f(inputs["w_in"])[0], "w_att_out": f(inputs["w_att_out"])[0], "w_conv_out": f(inputs["w_conv_out"])[0],
        "w_mix_out": f(inputs["w_mix_out"])[0], "w_cq": f(inputs["w_cq"])[0], "w_ck": f(inputs["w_ck"])[0],
        "w_cv": f(inputs["w_cv"])[0], "w_co": f(inputs["w_co"])[0], "w_pq": f(inputs["w_pq"])[0],
        "gcols": gcols, "convp": convp, "gfin": gfin, "biasT": biasT, "skT": skT, "expert_uT": euT, "expert_v": ev,
        "identf": np.eye(128, dtype=np.float32),
        "iota": np.ascontiguousarray(np.broadcast_to(np.arange(128, dtype=np.float32)[None, :], (128, 128))),
    }
    in_maps = []
    for c in range(8):
        bi, half = c // 2, c % 2
        xh = np.zeros((NTH, D), np.float32)
        if half == 1:
            xh[:] = x[bi, NT - HALO:2 * NT]
        else:
            xh[HALO:] = x[bi, 0:NT]
        m = dict(shared)
        m["xh"] = xh
        m["mem"] = memx[bi]
        m["hv"] = np.full((128, 1), float(half), np.float32)
        in_maps.append(m)
    return in_maps


def kernel(**inputs):
    in_maps = _host_prep(inputs)
    nc = build()
    res = run_bass_kernel_spmd(nc, in_maps, core_ids=list(range(8)))
    out = np.zeros((4, 4096, D), np.float32)
    for c in range(8):
        bi, half = c // 2, c % 2
        out[bi, half * NT:(half + 1) * NT] = res.results[c]["out"]
    return out
```
